# Optimizing a Trainium2 kernel written in Bass

```python
import jax, jax.numpy as jnp
from jax import lax
import numpy as np

D_MODEL = 1024
BATCH = 32
SEQ = 2048
DEPTH = 4

N_MIXERS = 3
N_MOD = 9
D_FF = 2816
EPS = 1e-6
RG_WIDTH = D_MODEL
RG_HEADS = 8
RG_BLOCK = RG_WIDTH // RG_HEADS
CONV_WIDTH = 4
RG_C = 8.0
HG_EXPAND = 128
HG_HEADS = D_MODEL // HG_EXPAND
GLA_HEADS = 4
GLA_KEY_DIM = D_MODEL // 2
GLA_VAL_DIM = D_MODEL
GLA_DK = GLA_KEY_DIM // GLA_HEADS
GLA_DV = GLA_VAL_DIM // GLA_HEADS
GLA_RANK = 16
GLA_LOGIT_NORM = 16.0
GLA_IN = 2 * GLA_KEY_DIM + 2 * GLA_VAL_DIM + GLA_RANK
CHUNK = 16

N_RG = len(range(0, DEPTH, N_MIXERS))
N_HG = len(range(1, DEPTH, N_MIXERS))
N_GLA = len(range(2, DEPTH, N_MIXERS))

kernel_name = 'hybrid_rglru_hgrn2_gla_macaron_adaln'


def rmsnorm(x, w):
    x32 = x.astype(jnp.float32)
    y = x32 * lax.rsqrt(jnp.mean(x32 * x32, axis=-1, keepdims=True) + EPS)
    return y.astype(x.dtype) * w


def modulate(h, shift, scale):
    return h * (1.0 + scale[:, None, :]) + shift[:, None, :]


def swiglu(h, w13, w2):
    gate, up = jnp.split(h @ w13, 2, axis=-1)
    return (jax.nn.silu(gate) * up) @ w2


def chunk_gla(q, k, v, log_f, scale):
    B, S, H, DK = q.shape
    DV = v.shape[-1]
    n = S // CHUNK

    def blocks(t):
        return t.astype(jnp.float32).reshape(B, n, CHUNK, H, t.shape[-1]).transpose(1, 0, 3, 2, 4)

    qb = blocks(q) * scale
    kb, vb = blocks(k), blocks(v)
    bcum = jnp.cumsum(blocks(log_f), axis=3)
    causal = jnp.tril(jnp.ones((CHUNK, CHUNK), bool))[:, :, None]

    def step(state, inp):
        q_c, k_c, v_c, b_c = inp
        o_inter = jnp.einsum('bhik,bhkv->bhiv', q_c * jnp.exp(b_c), state)
        rel = jnp.where(causal, b_c[:, :, :, None, :] - b_c[:, :, None, :, :], -jnp.inf)
        scores = jnp.einsum('bhik,bhjk,bhijk->bhij', q_c, k_c, jnp.exp(rel))
        o = o_inter + jnp.einsum('bhij,bhjv->bhiv', scores, v_c)
        b_last = b_c[:, :, -1:, :]
        state = (jnp.exp(b_last[:, :, 0, :, None]) * state
                 + jnp.einsum('bhjk,bhjv->bhkv', k_c * jnp.exp(b_last - b_c), v_c))
        return state, o

    state0 = jnp.zeros((B, H, DK, DV), jnp.float32)
    _, ob = lax.scan(step, state0, (qb, kb, vb, bcum))
    return ob.transpose(1, 0, 3, 2, 4).reshape(B, S, H, DV).astype(v.dtype)


def rglru_mixer(h, w_in, conv_w, conv_b, gate_w, gate_b, lam, w_out):
    B, S, _ = h.shape
    y_br, x_br = jnp.split(h @ w_in, 2, axis=-1)
    y_br = jax.nn.gelu(y_br)
    xp = jnp.pad(x_br, ((0, 0), (CONV_WIDTH - 1, 0), (0, 0)))
    xc = sum((xp[:, j:j + S] * conv_w[j] for j in range(CONV_WIDTH)), conv_b)
    xh = xc.reshape(B, S, RG_HEADS, RG_BLOCK)
    gates = jnp.einsum('bshi,hij->bshj', xh, gate_w) + gate_b
    gate_x, gate_a = jnp.split(gates.astype(jnp.float32), 2, axis=-1)
    gate_x = jax.nn.sigmoid(gate_x).reshape(B, S, RG_WIDTH)
    gate_a = jax.nn.sigmoid(gate_a).reshape(B, S, RG_WIDTH)
    log_a = -RG_C * gate_a * jax.nn.softplus(-lam.astype(jnp.float32))
    a = jnp.exp(log_a)
    mult = jnp.sqrt(-jnp.expm1(2.0 * log_a))
    mult = jnp.where(jnp.arange(S)[None, :, None] == 0, 1.0, mult)
    u = gate_x * xc.astype(jnp.float32) * mult

    def step(hc, inp):
        a_t, u_t = inp
        hn = a_t * hc + u_t
        return hn, hn

    _, hs = lax.scan(step, jnp.zeros((B, RG_WIDTH), jnp.float32),
                     (a.transpose(1, 0, 2), u.transpose(1, 0, 2)))
    hs = hs.transpose(1, 0, 2).astype(h.dtype)
    return (hs * y_br) @ w_out


def hgrn2_mixer(h, w_in, lb, norm_w, w_out):
    B, S, _ = h.shape
    q, fz, i_in, g = jnp.split(h @ w_in, 4, axis=-1)
    fz32 = fz.astype(jnp.float32)
    lb = lb.astype(jnp.float32)
    log_f = jnp.logaddexp(jnp.log(lb), jnp.log1p(-lb) + jax.nn.log_sigmoid(fz32))
    k = (1.0 - lb) * jax.nn.sigmoid(-fz32)
    heads = lambda t: t.reshape(B, S, HG_HEADS, HG_EXPAND)
    o = chunk_gla(heads(q), heads(k), heads(i_in), heads(log_f), 1.0)
    o = rmsnorm(o.reshape(B, S, D_MODEL), norm_w) * jax.nn.sigmoid(g)
    return o @ w_out


def gla_mixer(h, w_in, gate_w2, gate_b, norm_w, w_out):
    B, S, _ = h.shape
    q, k, v, g, a_low = jnp.split(
        h @ w_in,
        [GLA_KEY_DIM, 2 * GLA_KEY_DIM, 2 * GLA_KEY_DIM + GLA_VAL_DIM, 2 * GLA_KEY_DIM + 2 * GLA_VAL_DIM],
        axis=-1)
    log_a = jax.nn.log_sigmoid((a_low @ gate_w2 + gate_b).astype(jnp.float32)) / GLA_LOGIT_NORM
    kh = lambda t: t.reshape(B, S, GLA_HEADS, GLA_DK)
    vh = lambda t: t.reshape(B, S, GLA_HEADS, GLA_DV)
    o = chunk_gla(kh(q), kh(k), vh(v), kh(log_a), GLA_DK ** -0.5)
    o = rmsnorm(o, norm_w) * jax.nn.silu(vh(g))
    return o.reshape(B, S, GLA_VAL_DIM) @ w_out


def setup_inputs(seed: int = 0) -> dict:
    key = jax.random.key(seed)
    ks = jax.random.split(key, 24)
    nrm = lambda k, shape, s: jax.random.normal(k, shape, jnp.float32) * s
    u = jax.random.uniform(ks[13], (N_RG, RG_WIDTH), jnp.float32)
    a8 = 0.9 + 0.099 * u
    s = a8 ** (1.0 / RG_C)
    rg_lambda = jnp.log(s) - jnp.log1p(-s)
    return {
        'x': nrm(ks[0], (BATCH, SEQ, D_MODEL), 1.0),
        'c': nrm(ks[1], (BATCH, D_MODEL), 1.0),
        'ada_w': nrm(ks[2], (DEPTH, D_MODEL, N_MOD * D_MODEL), 0.5 * D_MODEL ** -0.5),
        'ada_b': nrm(ks[3], (DEPTH, N_MOD * D_MODEL), 0.01),
        'norm_w': 1.0 + nrm(ks[4], (DEPTH, 3, D_MODEL), 0.02),
        'final_norm_w': 1.0 + nrm(ks[5], (D_MODEL,), 0.02),
        'ffn_w13': nrm(ks[6], (DEPTH, 2, D_MODEL, 2 * D_FF), D_MODEL ** -0.5),
        'ffn_w2': nrm(ks[7], (DEPTH, 2, D_FF, D_MODEL), D_FF ** -0.5),
        'rg_w_in': nrm(ks[8], (N_RG, D_MODEL, 2 * RG_WIDTH), D_MODEL ** -0.5),
        'rg_conv_w': nrm(ks[9], (N_RG, CONV_WIDTH, RG_WIDTH), CONV_WIDTH ** -0.5),
        'rg_conv_b': nrm(ks[10], (N_RG, RG_WIDTH), 0.01),
        'rg_gate_w': nrm(ks[11], (N_RG, RG_HEADS, RG_BLOCK, 2 * RG_BLOCK), RG_BLOCK ** -0.5),
        'rg_gate_b': nrm(ks[12], (N_RG, RG_HEADS, 2 * RG_BLOCK), 0.01),
        'rg_lambda': rg_lambda,
        'rg_w_out': nrm(ks[14], (N_RG, RG_WIDTH, D_MODEL), RG_WIDTH ** -0.5),
        'hg_w_in': nrm(ks[15], (N_HG, D_MODEL, 4 * D_MODEL), D_MODEL ** -0.5),
        'hg_lb_logits': nrm(ks[16], (DEPTH, D_MODEL), 0.1),
        'hg_norm_w': 1.0 + nrm(ks[17], (N_HG, D_MODEL), 0.02),
        'hg_w_out': nrm(ks[18], (N_HG, D_MODEL, D_MODEL), D_MODEL ** -0.5),
        'gla_w_in': nrm(ks[19], (N_GLA, D_MODEL, GLA_IN), D_MODEL ** -0.5),
        'gla_gate_w2': nrm(ks[20], (N_GLA, GLA_RANK, GLA_KEY_DIM), GLA_RANK ** -0.5),
        'gla_gate_b': nrm(ks[21], (N_GLA, GLA_KEY_DIM), 0.01),
        'gla_norm_w': 1.0 + nrm(ks[22], (N_GLA, GLA_DV), 0.02),
        'gla_w_out': nrm(ks[23], (N_GLA, GLA_VAL_DIM, D_MODEL), GLA_VAL_DIM ** -0.5),
    }


def reference(x, c, ada_w, ada_b, norm_w, final_norm_w, ffn_w13, ffn_w2,
              rg_w_in, rg_conv_w, rg_conv_b, rg_gate_w, rg_gate_b, rg_lambda, rg_w_out,
              hg_w_in, hg_lb_logits, hg_norm_w, hg_w_out,
              gla_w_in, gla_gate_w2, gla_gate_b, gla_norm_w, gla_w_out):
    c_act = jax.nn.silu(c)
    p = jax.nn.softmax(hg_lb_logits.astype(jnp.float32), axis=0)
    lower_bounds = jnp.cumsum(p, axis=0) - p[0]
    i_rg = i_hg = i_gla = 0
    for l in range(DEPTH):
        mod = c_act @ ada_w[l] + ada_b[l]
        sh1, sc1, g1, sh2, sc2, g2, sh3, sc3, g3 = jnp.split(mod, N_MOD, axis=-1)
        h = modulate(rmsnorm(x, norm_w[l, 0]), sh1, sc1)
        x = x + 0.5 * g1[:, None, :] * swiglu(h, ffn_w13[l, 0], ffn_w2[l, 0])
        h = modulate(rmsnorm(x, norm_w[l, 1]), sh2, sc2)
        m = l % N_MIXERS
        if m == 0:
            y = rglru_mixer(h, rg_w_in[i_rg], rg_conv_w[i_rg], rg_conv_b[i_rg], rg_gate_w[i_rg],
                            rg_gate_b[i_rg], rg_lambda[i_rg], rg_w_out[i_rg])
            i_rg += 1
        elif m == 1:
            y = hgrn2_mixer(h, hg_w_in[i_hg], lower_bounds[l], hg_norm_w[i_hg], hg_w_out[i_hg])
            i_hg += 1
        else:
            y = gla_mixer(h, gla_w_in[i_gla], gla_gate_w2[i_gla], gla_gate_b[i_gla],
                          gla_norm_w[i_gla], gla_w_out[i_gla])
            i_gla += 1
        x = x + g2[:, None, :] * y
        h = modulate(rmsnorm(x, norm_w[l, 2]), sh3, sc3)
        x = x + 0.5 * g3[:, None, :] * swiglu(h, ffn_w13[l, 1], ffn_w2[l, 1])
    return rmsnorm(x, final_norm_w)
```

```python
import numpy as np
import concourse.bass as bass
import concourse.mybir as mybir
from concourse.bass_utils import run_bass_kernel_spmd

F32 = mybir.dt.float32
BF16 = mybir.dt.bfloat16
AF = mybir.ActivationFunctionType
ALU = mybir.AluOpType
AX = mybir.AxisListType

D = 1024
DC = 8
DFF = 2816
FC = 22
SEQ = 2048
NCORES = 8
SEQ_PER_CORE = 4
TB = 1024
NT = TB // 512
NBLK = SEQ_PER_CORE * SEQ // TB
BLK_PER_SEQ = SEQ // TB
CH = 64
NCL = 512 // CH
EPS = 1e-6
DEPTH = 4
NS = 4
SLOT = 4096
LIVE = 2
ARENA_WORDS = 16384

SAME_ENGINE_SYNC = True


class SemW:
    def __init__(self, nc, name):
        self.sem = nc.alloc_semaphore(name)
        self.count = 0


class Tk:
    __slots__ = ("writer", "readers")

    def __init__(self):
        self.writer = None
        self.readers = {}


class _Rec:
    def __init__(self):
        self.calls = []

    def __getattr__(self, name):
        def f(*a, **kw):
            self.calls.append((name, a, kw))
            return self
        return f


class K:
    ENGS = ("pe", "act", "dve", "pool", "sp")
    CE = ("pe", "act", "dve", "pool")

    def __init__(self, nc):
        self.nc = nc
        self.dry = False
        self.streams = {e: [] for e in self.ENGS}
        self.esem = {e: SemW(nc, "s_" + e) for e in self.CE}
        self.pending = {e: False for e in self.CE}
        self.waited = {e: {} for e in self.ENGS}
        self.rings = {"sp": [SemW(nc, "dsp%d" % i) for i in range(8)],
                      "pool": [SemW(nc, "dpl%d" % i) for i in range(8)],
                      "act": [SemW(nc, "dac%d" % i) for i in range(4)]}
        self.ridx = {"sp": 0, "pool": 0, "act": 0}
        self.n_instr = {e: 0 for e in self.ENGS}

    def _wait(self, eng, semw, val):
        w = self.waited[eng]
        if w.get(id(semw), 0) >= val:
            return
        w[id(semw)] = val
        sem = semw.sem
        self.streams[eng].append(lambda e, sem=sem, val=val: e.wait_ge(sem, val))

    def _deps(self, eng, reads, writes, is_dma=False):
        for t in reads:
            wr = t.writer
            if wr is not None:
                if wr[2] != eng or is_dma or (SAME_ENGINE_SYNC and eng != "pe"):
                    self._wait(eng, wr[0], wr[1])
        for t in writes:
            wr = t.writer
            if wr is not None and (wr[2] != eng or is_dma):
                self._wait(eng, wr[0], wr[1])
            for en, (s, v) in t.readers.items():
                if en != eng or is_dma:
                    self._wait(eng, s, v)

    def _mark(self, eng, semw, val, reads, writes):
        for t in reads:
            t.readers[eng] = (semw, val)
        for t in writes:
            t.writer = (semw, val, eng)
            t.readers = {}

    def op(self, eng, fn, reads=(), writes=(), inc=True):
        if self.dry:
            return
        self._deps(eng, reads, writes)
        semw = self.esem[eng]
        val = semw.count + 1
        rec = _Rec()
        fn(rec)
        assert len(rec.calls) == 1
        name, a, kw = rec.calls[0]
        if inc:
            semw.count = val
            self.pending[eng] = False
            sem = semw.sem
            self.streams[eng].append(lambda e, name=name, a=a, kw=kw, sem=sem: getattr(e, name)(*a, **kw).then_inc(sem, 1))
        else:
            self.pending[eng] = True
            self.streams[eng].append(lambda e, name=name, a=a, kw=kw: getattr(e, name)(*a, **kw))
        self.n_instr[eng] += 1
        self._mark(eng, semw, val, reads, writes)

    def dma(self, q, out, in_, reads=(), writes=(), **kw):
        if self.dry:
            return
        ring = self.rings[q]
        semw = ring[self.ridx[q]]
        self.ridx[q] = (self.ridx[q] + 1) % len(ring)
        if semw.count > 0:
            self._wait(q, semw, semw.count)
        self._deps(q, reads, writes, is_dma=True)
        semw.count += 16
        val = semw.count
        sem = semw.sem
        self.streams[q].append(
            lambda e, out=out, in_=in_, sem=sem, kw=kw: e.dma_start(out=out, in_=in_, **kw).then_inc(sem, 16))
        self.n_instr[q] += 1
        self._mark("dma%d" % id(semw), semw, val, reads, writes)

    def wait_all(self, eng, toks):
        if self.dry:
            return
        for t in toks:
            if t.writer is not None:
                self._wait(eng, t.writer[0], t.writer[1])

    def barrier(self, engs=("pe", "act", "dve")):
        if self.dry:
            return
        for e in engs:
            assert not self.pending[e]
        for e in engs:
            for o in engs:
                if o != e and self.esem[o].count > 0:
                    self._wait(e, self.esem[o], self.esem[o].count)

    def finish(self):
        nc = self.nc
        for e in self.CE:
            assert not self.pending[e], "engine %s has un-incremented trailing ops" % e
        st = self.streams
        with nc.Block() as block:
            @block.sync
            def _(e):
                for f in st["sp"]:
                    f(e)

            @block.tensor
            def _(e):
                for f in st["pe"]:
                    f(e)

            @block.scalar
            def _(e):
                for f in st["act"]:
                    f(e)

            @block.vector
            def _(e):
                for f in st["dve"]:
                    f(e)

            @block.gpsimd
            def _(e):
                for f in st["pool"]:
                    f(e)


class WStream:
    def __init__(self, k, nc):
        self.k = k
        self.slots = [nc.alloc_sbuf_tensor("wslot%d" % i, [128, SLOT], BF16) for i in range(NS)]
        self.tk = [Tk() for _ in range(NS)]
        self.plan = []
        self.pos = 0
        self.issued = 0

    def _view(self, s, src):
        a, b = src.shape[1], src.shape[2]
        assert a * b <= SLOT
        return self.slots[s][:, 0:a * b].rearrange("p (a b) -> p a b", a=a)

    def get(self, src):
        if self.k.dry:
            self.plan.append(src)
            return self._view(0, src), self.tk[0]
        i = self.pos
        self.pos += 1
        lim = min(len(self.plan), i + NS - LIVE + 1)
        while self.issued < lim:
            j = self.issued
            s = j % NS
            sj = self.plan[j]
            self.k.dma("pool", self._view(s, sj), sj, writes=[self.tk[s]])
            self.issued += 1
        s = i % NS
        return self._view(s, src), self.tk[s]


class Arena:
    def __init__(self, nc):
        self.t = nc.alloc_sbuf_tensor("arena", [128, ARENA_WORDS], F32)
        self.off = 0

    def reset(self, off=0):
        self.off = off

    def f32(self, n):
        a = self.off
        self.off += n
        assert self.off <= ARENA_WORDS, "arena overflow %d" % self.off
        return self.t[:, a:a + n]

    def bf16(self, n):
        w = (n + 1) // 2
        a = self.off
        self.off += w
        assert self.off <= ARENA_WORDS, "arena overflow %d" % self.off
        return self.t[:, a:a + w].bitcast(BF16)[:, 0:n]


class Prog:
    def __init__(self, layers, first_launch, last_launch, nblk=NBLK):
        self.layers = list(layers)
        self.li = {l: i for i, l in enumerate(self.layers)}
        nl = len(self.layers)
        self.parts = ('ffn0', 'mix', 'ffn1')
        self.final = last_launch
        self.nblk = nblk
        nc = bass.Bass("TRN2", target_bir_lowering=False)
        self.nc = nc
        ntok = nblk * TB
        nseq = max(1, ntok // SEQ)
        self.nseq = nseq
        dt = lambda name, shape: nc.dram_tensor(name, shape, F32, kind="ExternalInput").ap()
        self.x = dt("x", [ntok, D])
        self.c = dt("c", [SEQ_PER_CORE, D])
        self.ada_w = dt("ada_w", [nl, D, 9 * D])
        self.ada_b = dt("ada_b", [DEPTH, 9 * D])
        self.norm_w = dt("norm_w", [DEPTH * 3, D])
        self.final_norm_w = dt("final_norm_w", [1, D])
        self.ffn_w13 = dt("ffn_w13", [nl, 2, D, 2 * DFF])
        self.ffn_w2 = dt("ffn_w2", [nl, 2, DFF, D])
        self.rg_w_in = dt("rg_w_in", [2, D, 2 * D])
        self.rg_conv_w = dt("rg_conv_w", [8, D])
        self.rg_conv_b = dt("rg_conv_b", [2, D])
        self.rg_gate_w = dt("rg_gate_w", [2, 8, 128, 256])
        self.rg_gate_b = dt("rg_gate_b", [32, 128])
        self.rg_lambda = dt("rg_lambda", [2, D])
        self.rg_w_out = dt("rg_w_out", [2, D, D])
        self.hg_w_in = dt("hg_w_in", [1, D, 4 * D])
        self.hg_lb_logits = dt("hg_lb_logits", [DEPTH, D])
        self.hg_norm_w = dt("hg_norm_w", [1, D])
        self.hg_w_out = dt("hg_w_out", [1, D, D])
        self.gla_w_in = dt("gla_w_in", [1, D, 3088])
        self.gla_gate_w2 = dt("gla_gate_w2", [16, 512])
        self.gla_gate_b = dt("gla_gate_b", [4, 128])
        self.gla_norm_w = dt("gla_norm_w", [2, 128])
        self.gla_w_out = dt("gla_w_out", [1, D, D])
        self.y = nc.dram_tensor("y", [ntok, D], F32, kind="ExternalOutput").ap()

        self._acache = {}
        self.k = K(nc)
        self.W = WStream(self.k, nc)
        self.ar = Arena(nc)
        sb = nc.alloc_sbuf_tensor
        self.xT = sb("xT", [128, DC, TB], F32)
        self.hT = sb("hT", [128, DC, TB], BF16)
        self.X = [[Tk() for _ in range(NT)] for _ in range(DC)]
        self.Hh = [[Tk() for _ in range(NT)] for _ in range(DC)]
        self.psb = [nc.alloc_psum_tensor("psb%d" % i, [128, 1024], F32) for i in range(4)]
        self.PS = [Tk() for _ in range(8)]
        self.identf = sb("identf", [128, 128], F32)
        self.identb = sb("identb", [128, 128], BF16)
        self.onesb = sb("onesb", [128, 128], BF16)
        self.mask8 = sb("mask8", [CH, 8 * CH], F32)
        self.cmask = sb("cmask", [128, 512], F32)
        self.CONST = Tk()
        self.sq = sb("sq", [128, DC, 512], BF16)
        self.SQ = [Tk() for _ in range(DC)]
        self.ntmp = [sb("ntmp%d" % i, [128, 512], F32) for i in range(2)]
        self.NTMP = [Tk(), Tk()]
        self.rstd = sb("rstd", [128, 512], F32)
        self.RSTD = Tk()
        self.xio = [sb("xio%d" % i, [128, D], F32) for i in range(2)]
        self.XIO = [Tk(), Tk()]
        self.cactT = sb("cactT", [128, DC, SEQ_PER_CORE], BF16)
        self.mods = {l: sb("mods%d" % l, [128, 72, SEQ_PER_CORE], F32) for l in self.layers}
        self.Amod = {l: sb("Amod%d" % l, [128, SEQ_PER_CORE, 3, DC], F32) for l in self.layers}
        self.Gmod = {l: sb("Gmod%d" % l, [128, SEQ_PER_CORE, 3, DC], F32) for l in self.layers}
        self.rg_h = sb("rg_h", [128, 2, DC], F32)
        self.rg_tail = sb("rg_tail", [128, 2, DC, 3], F32)
        self.RGH = [[Tk() for _ in range(DC)] for _ in range(2)]
        self.RGT = [[Tk() for _ in range(DC)] for _ in range(2)]
        self.st = {}
        self.stb = {}
        self.ST = {}
        self.STB = {}
        for kind in ("hg", "gla"):
            self.st[kind] = sb("st_" + kind, [128, 1024], F32)
            self.stb[kind] = sb("stb_" + kind, [128, 1024], BF16)
            self.ST[kind] = Tk()
            self.STB[kind] = Tk()
        self.gatew = sb("gatew", [128, 2, 8, 256], BF16)
        self.gw2 = sb("gw2", [16, 512], BF16)
        self.ps_rot = 0

    def bank(self, i):
        return self.psb[i // 2][:, (i % 2) * 512:(i % 2) * 512 + 512]

    def bankb(self, i):
        return self.psb[i // 2][:, (i % 2) * 512:(i % 2) * 512 + 512].bitcast(BF16)

    def mm_group(self, out, pairs, out_tk, reads, per_reads=None):
        n = len(pairs)
        for i, (lhsT, rhs) in enumerate(pairs):
            rd = list(reads) if i == 0 else []
            if per_reads is not None:
                rd = rd + list(per_reads[i])
            self.k.op("pe", lambda e, lhsT=lhsT, rhs=rhs, i=i: e.matmul(out, lhsT, rhs, start=(i == 0), stop=(i == n - 1)),
                      reads=rd, writes=[out_tk], inc=(i == n - 1))

    def vecT(self, name, src2d, R, Wd, func=None, dtype=F32):
        k = self.k
        nch = Wd // 128
        assert nch * R <= 512
        stage = self.ar.f32(Wd)[0:R, :]
        tk = Tk()
        k.dma("sp", stage, src2d, writes=[tk])
        if func is not None:
            k.op("act", lambda e: e.activation(stage, stage, func), reads=[tk], writes=[tk])
        b = self.ps_rot % 6
        self.ps_rot += 1
        ps = self.bank(b)
        for c in range(nch):
            k.op("pe", lambda e, c=c: e.transpose(ps[:, c * R:(c + 1) * R], stage[:, c * 128:(c + 1) * 128], self.identf[0:R, 0:R]),
                 reads=[tk, self.CONST], writes=[self.PS[b]], inc=(c == nch - 1))
        dst = self.alloc(name, [128, nch, R], dtype)
        k.op("dve", lambda e: e.tensor_copy(dst[:], ps[:, 0:nch * R].rearrange("p (c r) -> p c r", c=nch)),
             reads=[self.PS[b]], writes=[self.CONST])
        return dst

    def prologue(self):
        k, nc = self.k, self.nc
        C = self.CONST
        k.op("pool", lambda e: e.memset(self.identf[:], 0.0), writes=[C])
        k.op("pool", lambda e: e.affine_select(self.identf[:], self.identf[:], pattern=[[-1, 128]], compare_op=ALU.not_equal,
                                               fill=1.0, base=0, channel_multiplier=1), reads=[C], writes=[C])
        k.op("pool", lambda e: e.tensor_copy(self.identb[:], self.identf[:]), reads=[C], writes=[C])
        k.op("pool", lambda e: e.memset(self.onesb[:], 1.0), writes=[C])
        k.op("pool", lambda e: e.memset(self.mask8[:], 1.0), writes=[C])
        for h in range(8):
            k.op("pool", lambda e, h=h: e.affine_select(self.mask8[:, h * CH:(h + 1) * CH], self.mask8[:, h * CH:(h + 1) * CH],
                                                        pattern=[[1, CH]], compare_op=ALU.is_ge, fill=0.0, base=0,
                                                        channel_multiplier=-1), reads=[C], writes=[C])
        k.op("pool", lambda e: e.memset(self.cmask[:], 1.0), writes=[C])
        k.op("pool", lambda e: e.memset(self.cmask[:].rearrange("p (a b) -> p a b", b=CH)[:, :, 0:1], 0.0), reads=[C], writes=[C])
        k.dma("pool", self.gatew[:], self.rg_gate_w.rearrange("i h p j -> p i h j"), writes=[C])
        k.dma("pool", self.gw2[:], self.gla_gate_w2, writes=[C])
        k.barrier(("pe", "act", "dve", "pool"))
        self.ar.reset()
        cT = self.vecT("cTf", self.c, SEQ_PER_CORE, D, func=AF.Silu)
        k.op("dve", lambda e: e.tensor_copy(self.cactT[:], cT[:]), reads=[C], writes=[C])
        self.normwT = self.vecT("normwT", self.norm_w, 12, D)
        self.fnwT = self.vecT("fnwT", self.final_norm_w, 1, D)
        self.convwT = self.vecT("convwT", self.rg_conv_w, 8, D)
        self.convbT = self.vecT("convbT", self.rg_conv_b, 2, D)
        self.lamT = self.vecT("lamT", self.rg_lambda, 2, D)
        self.gbT = self.vecT("gbT", self.rg_gate_b, 32, 128)
        self.lbgT = self.vecT("lbgT", self.hg_lb_logits, DEPTH, D)
        self.hgnwT = self.vecT("hgnwT", self.hg_norm_w, 1, D)
        self.glabT = self.vecT("glabT", self.gla_gate_b, 4, 128)
        self.glanwT = self.vecT("glanwT", self.gla_norm_w, 2, 128)
        self.adabT = {}
        for l in self.layers:
            self.adabT[l] = self.vecT("adabT%d" % l, self.ada_b[l].rearrange("(j p) -> j p", p=128), 72, 128)
        sb = self.alloc
        self.cA = sb("cA", [128, DC, 2], F32)
        t = [sb("pt%d" % i, [128, DC, 4], F32) for i in range(6)]
        v = lambda x: x[:, :, 0:2]
        lam = self.lamT[:]
        k.op("act", lambda e: e.activation(v(t[0]), lam, AF.Abs), reads=[C], writes=[C])
        k.op("act", lambda e: e.activation(v(t[1]), v(t[0]), AF.Exp, scale=-1.0), reads=[C], writes=[C])
        k.op("dve", lambda e: e.tensor_scalar(v(t[2]), v(t[1]), 1.0, None, ALU.add), reads=[C], writes=[C])
        k.op("act", lambda e: e.activation(v(t[3]), v(t[2]), AF.Ln), reads=[C], writes=[C])
        k.op("dve", lambda e: e.tensor_scalar(v(t[2]), v(t[2]), -1.0, 1e-30, ALU.add, ALU.max), reads=[C], writes=[C])
        k.op("dve", lambda e: e.reciprocal(v(t[2]), v(t[2])), reads=[C], writes=[C])
        k.op("dve", lambda e: e.tensor_tensor(v(t[3]), v(t[3]), v(t[1]), ALU.mult), reads=[C], writes=[C])
        k.op("dve", lambda e: e.tensor_tensor(v(t[3]), v(t[3]), v(t[2]), ALU.mult), reads=[C], writes=[C])
        k.op("dve", lambda e: e.tensor_scalar(v(t[4]), lam, -1.0, 0.0, ALU.mult, ALU.max), reads=[C], writes=[C])
        k.op("dve", lambda e: e.tensor_tensor(v(t[4]), v(t[4]), v(t[3]), ALU.add), reads=[C], writes=[C])
        k.op("dve", lambda e: e.tensor_scalar(self.cA[:], v(t[4]), -8.0, None, ALU.mult), reads=[C], writes=[C])
        self.cA2 = sb("cA2", [128, DC, 2], F32)
        k.op("dve", lambda e: e.tensor_scalar(self.cA2[:], v(t[4]), -16.0, None, ALU.mult), reads=[C], writes=[C])
        self.lb = sb("lb", [128, DC, DEPTH], F32)
        self.oml = sb("oml", [128, DC, DEPTH], F32)
        self.noml = sb("noml", [128, DC, DEPTH], F32)
        g = self.lbgT[:]
        mx = sb("lbmx", [128, DC], F32)
        ex = sb("lbex", [128, DC, DEPTH], F32)
        sm = sb("lbsm", [128, DC], F32)
        k.op("dve", lambda e: e.tensor_reduce(mx[:], g, AX.X, ALU.max), reads=[C], writes=[C])
        k.op("dve", lambda e: e.tensor_tensor(ex[:], g, mx[:].unsqueeze(2).to_broadcast([128, DC, DEPTH]), ALU.subtract), reads=[C], writes=[C])
        k.op("act", lambda e: e.activation(ex[:], ex[:], AF.Exp), reads=[C], writes=[C])
        k.op("dve", lambda e: e.tensor_reduce(sm[:], ex[:], AX.X, ALU.add), reads=[C], writes=[C])
        k.op("dve", lambda e: e.reciprocal(sm[:], sm[:]), reads=[C], writes=[C])
        k.op("dve", lambda e: e.memset(self.lb[:], 0.0), reads=[C], writes=[C])
        for l in range(1, DEPTH):
            k.op("dve", lambda e, l=l: e.tensor_tensor(self.lb[:, :, l], self.lb[:, :, l - 1], ex[:, :, l], ALU.add), reads=[C], writes=[C])
        k.op("dve", lambda e: e.tensor_tensor(self.lb[:], self.lb[:], sm[:].unsqueeze(2).to_broadcast([128, DC, DEPTH]), ALU.mult), reads=[C], writes=[C])
        k.op("dve", lambda e: e.tensor_scalar(self.oml[:], self.lb[:], -1.0, 1.0, ALU.mult, ALU.add), reads=[C], writes=[C])
        k.op("dve", lambda e: e.tensor_scalar(self.noml[:], self.oml[:], -1.0, None, ALU.mult), reads=[C], writes=[C])
        for l in self.layers:
            awv = self.ada_w[self.li[l]].rearrange("(k p) n -> p k n", p=128)
            ps = self.bank(7)
            for grp in range(18):
                wt, wtk = self.W.get(awv[:, :, grp * 512:(grp + 1) * 512])
                for jj in range(4):
                    j = grp * 4 + jj
                    self.mm_group(ps[:, j * 4:(j + 1) * 4],
                                  [(wt[:, kk, jj * 128:(jj + 1) * 128], self.cactT[:, kk, :]) for kk in range(DC)],
                                  self.PS[7], [wtk, C])
            md = self.mods[l]
            k.op("dve", lambda e, md=md, l=l: e.tensor_tensor(md[:], ps[:, 0:288].rearrange("p (j s) -> p j s", s=SEQ_PER_CORE),
                                                              self.adabT[l][:, 0, :].unsqueeze(2).to_broadcast([128, 72, SEQ_PER_CORE]), ALU.add),
                 reads=[self.PS[7], C], writes=[C])
            for s in range(SEQ_PER_CORE):
                for sub in range(3):
                    sc = md[:, (3 * sub + 1) * 8:(3 * sub + 2) * 8, s]
                    gt = md[:, (3 * sub + 2) * 8:(3 * sub + 3) * 8, s]
                    k.op("dve", lambda e, sc=sc, l=l, s=s, sub=sub: e.scalar_tensor_tensor(
                        self.Amod[l][:, s, sub, :], sc, 1.0, self.normwT[:, :, l * 3 + sub], ALU.add, ALU.mult), reads=[C], writes=[C])
                    k.op("dve", lambda e, gt=gt, l=l, s=s, sub=sub: e.tensor_scalar(
                        self.Gmod[l][:, s, sub, :], gt, (1.0 if sub == 1 else 0.5), None, ALU.mult), reads=[C], writes=[C])
        k.barrier(("pe", "act", "dve", "pool"))

    def emit_load(self, blk):
        k = self.k
        for t8 in range(TB // 128):
            xi = self.xio[t8 % 2]
            tki = self.XIO[t8 % 2]
            k.dma("sp", xi[:], self.x[blk * TB + t8 * 128: blk * TB + (t8 + 1) * 128, :], writes=[tki])
            tt = t8 // 4
            for half in range(2):
                b = half
                ps = self.bank(b)
                for cc in range(4):
                    c = half * 4 + cc
                    k.op("pe", lambda e, cc=cc, c=c, ps=ps, xi=xi: e.transpose(ps[:, cc * 128:(cc + 1) * 128], xi[:, c * 128:(c + 1) * 128], self.identf[:]),
                         reads=[tki, self.CONST], writes=[self.PS[b]], inc=(cc == 3))
                dst = self.xT[:, half * 4:(half + 1) * 4, t8 * 128:(t8 + 1) * 128]
                eng = "act" if half == 0 else "dve"
                if eng == "act":
                    k.op("act", lambda e, dst=dst, ps=ps: e.activation(dst, ps.rearrange("p (c t) -> p c t", c=4), AF.Copy),
                         reads=[self.PS[b]], writes=[self.X[half * 4 + cc][tt] for cc in range(4)])
                else:
                    k.op("dve", lambda e, dst=dst, ps=ps: e.tensor_copy(dst, ps.rearrange("p (c t) -> p c t", c=4)),
                         reads=[self.PS[b]], writes=[self.X[half * 4 + cc][tt] for cc in range(4)])

    def rstd_tile(self, src_of_c, src_tks, tt, nchunks, bankid, scale, in_psum=False):
        k = self.k
        for c in range(nchunks):
            k.op("act", lambda e, c=c: e.activation(self.sq[:, c, :], src_of_c(c), AF.Square), reads=[src_tks[c]], writes=[self.SQ[c]])
        ps = self.bank(bankid)
        self.mm_group(ps, [(self.onesb[:], self.sq[:, c, :]) for c in range(nchunks)], self.PS[bankid],
                      [self.CONST], per_reads=[[self.SQ[c]] for c in range(nchunks)])
        if in_psum:
            k.op("act", lambda e: e.activation(ps, ps, AF.Sqrt, scale=scale, bias=EPS), reads=[self.PS[bankid]], writes=[self.PS[bankid]])
            k.op("dve", lambda e: e.reciprocal(ps, ps), reads=[self.PS[bankid]], writes=[self.PS[bankid]])
        else:
            k.op("act", lambda e: e.activation(self.rstd[:], ps, AF.Sqrt, scale=scale, bias=EPS), reads=[self.PS[bankid]], writes=[self.RSTD])
            k.op("dve", lambda e: e.reciprocal(self.rstd[:], self.rstd[:]), reads=[self.RSTD], writes=[self.RSTD])

    def emit_store(self, blk, final):
        k = self.k
        for tt in range(NT):
            if final:
                self.rstd_tile(lambda c: self.xT[:, c, tt * 512:(tt + 1) * 512], [self.X[c][tt] for c in range(DC)], tt, DC, 6 + tt, 1.0 / D)
            for t4 in range(4):
                t0 = tt * 512 + t4 * 128
                xo = self.xio[t4 % 2]
                tko = self.XIO[t4 % 2]
                for half in range(2):
                    b = half
                    ps = self.bank(b)
                    for cc in range(4):
                        c = half * 4 + cc
                        if final:
                            nt_ = self.ntmp[cc % 2]
                            ntk = self.NTMP[cc % 2]
                            k.op("dve", lambda e, c=c, nt_=nt_: e.scalar_tensor_tensor(
                                nt_[:, 0:128], self.xT[:, c, t0:t0 + 128], self.fnwT[:, c, 0:1], self.rstd[:, t4 * 128:(t4 + 1) * 128], ALU.mult, ALU.mult),
                                reads=[self.X[c][tt], self.RSTD, self.CONST], writes=[ntk])
                            src = nt_[:, 0:128]
                            rd = [ntk, self.CONST]
                        else:
                            src = self.xT[:, c, t0:t0 + 128]
                            rd = [self.X[c][tt], self.CONST]
                        k.op("pe", lambda e, cc=cc, ps=ps, src=src: e.transpose(ps[:, cc * 128:(cc + 1) * 128], src, self.identf[:]),
                             reads=rd, writes=[self.PS[b]])
                    if half == 0:
                        k.op("act", lambda e, xo=xo, ps=ps: e.activation(xo[:, 0:512], ps, AF.Copy), reads=[self.PS[b]], writes=[tko])
                    else:
                        k.op("dve", lambda e, xo=xo, ps=ps: e.tensor_copy(xo[:, 512:1024], ps), reads=[self.PS[b]], writes=[tko])
                yt = Tk()
                k.dma("sp", self.y[blk * TB + t0: blk * TB + t0 + 128, :], xo[:], reads=[tko], writes=[yt])
                self.out_tks.append(yt)

    def emit_norm(self, l, sub, s):
        k = self.k
        for tt in range(NT):
            sl = slice(tt * 512, (tt + 1) * 512)
            b = 6 + tt
            self.rstd_tile(lambda c: self.xT[:, c, sl], [self.X[c][tt] for c in range(DC)], tt, DC, b, 1.0 / D, in_psum=True)
            for c in range(DC):
                nt_ = self.ntmp[c % 2]
                ntk = self.NTMP[c % 2]
                k.op("dve", lambda e: e.tensor_tensor(nt_[:], self.xT[:, c, sl], self.bank(b), ALU.mult),
                     reads=[self.X[c][tt], self.PS[b]], writes=[ntk])
                k.op("act", lambda e: e.activation(self.hT[:, c, sl], nt_[:], AF.Identity, scale=self.Amod[l][:, s, sub, c:c + 1],
                                                   bias=self.mods[l][:, 3 * sub * 8 + c, s:s + 1]),
                     reads=[ntk, self.CONST], writes=[self.Hh[c][tt]])

    def x_update(self, ps, psk, l, s, sub, m, tt):
        dst = self.xT[:, m, tt * 512:(tt + 1) * 512]
        self.k.op("dve", lambda e: e.scalar_tensor_tensor(dst, ps, self.Gmod[l][:, s, sub, m:m + 1], dst, ALU.mult, ALU.add),
                  reads=[psk, self.X[m][tt], self.CONST], writes=[self.X[m][tt]])

    def out_proj(self, wov, src, src_tks, l, s, sub):
        for mg in range(2):
            wt, wtk = self.W.get(wov[:, :, mg * 512:(mg + 1) * 512])
            for mm in range(4):
                m = mg * 4 + mm
                for tt in range(NT):
                    b = (m * NT + tt) % 4
                    ps = self.bank(b)
                    self.mm_group(ps, [(wt[:, kk, mm * 128:(mm + 1) * 128], src[:, kk, tt * 512:(tt + 1) * 512]) for kk in range(DC)],
                                  self.PS[b], [wtk] + [src_tks[kk][tt] for kk in range(DC)])
                    self.x_update(ps, self.PS[b], l, s, sub, m, tt)

    def emit_ffn(self, l, f, s):
        k = self.k
        sub = 0 if f == 0 else 2
        k.barrier()
        self.emit_norm(l, sub, s)
        ar = self.ar
        ar.reset()
        aT = ar.bf16(FC * TB).rearrange("p (j t) -> p j t", j=FC)
        AT = [[Tk() for _ in range(NT)] for _ in range(FC)]
        sg = [ar.f32(512) for _ in range(2)]
        SG = [Tk(), Tk()]
        w13v = self.ffn_w13[self.li[l], f].rearrange("(k p) n -> p k n", p=128)
        rot = 0
        for j0 in range(0, FC, 4):
            nj = min(4, FC - j0)
            gt, gtk = self.W.get(w13v[:, :, j0 * 128:(j0 + nj) * 128])
            ut, utk = self.W.get(w13v[:, :, DFF + j0 * 128:DFF + (j0 + nj) * 128])
            for jj in range(nj):
                j = j0 + jj
                for tt in range(NT):
                    sl = slice(tt * 512, (tt + 1) * 512)
                    bg, bu = (rot % 2) * 2, (rot % 2) * 2 + 1
                    rot += 1
                    hr = [self.Hh[kk][tt] for kk in range(DC)]
                    self.mm_group(self.bank(bg), [(gt[:, kk, jj * 128:(jj + 1) * 128], self.hT[:, kk, sl]) for kk in range(DC)], self.PS[bg], [gtk] + hr)
                    self.mm_group(self.bank(bu), [(ut[:, kk, jj * 128:(jj + 1) * 128], self.hT[:, kk, sl]) for kk in range(DC)], self.PS[bu], [utk] + hr)
                    sgi = rot % 2
                    k.op("act", lambda e, bg=bg, sgi=sgi: e.activation(sg[sgi], self.bank(bg), AF.Silu), reads=[self.PS[bg]], writes=[SG[sgi]])
                    k.op("dve", lambda e, bu=bu, sgi=sgi, j=j, sl=sl: e.tensor_tensor(aT[:, j, sl], sg[sgi], self.bank(bu), ALU.mult),
                         reads=[SG[sgi], self.PS[bu]], writes=[AT[j][tt]])
        w2v = self.ffn_w2[self.li[l], f].rearrange("(j p) n -> p j n", p=128)
        for mg in range(4):
            wa, wak = self.W.get(w2v[:, 0:11, mg * 256:(mg + 1) * 256])
            wb, wbk = self.W.get(w2v[:, 11:22, mg * 256:(mg + 1) * 256])
            for mm in range(2):
                m = mg * 2 + mm
                for tt in range(NT):
                    sl = slice(tt * 512, (tt + 1) * 512)
                    b = 4 + (m * NT + tt) % 2
                    pairs = [((wa if j < 11 else wb)[:, j % 11, mm * 128:(mm + 1) * 128], aT[:, j, sl]) for j in range(FC)]
                    self.mm_group(self.bank(b), pairs, self.PS[b], [wak, wbk] + [AT[j][tt] for j in range(FC)])
                    self.x_update(self.bank(b), self.PS[b], l, s, sub, m, tt)

    def _rg_chain(self, i, hd, hl, tt, first0, B, banks, xt_, xtk, yt_, ytk, hy, HY):
        k = self.k
        C = self.CONST
        xpad, ty, xc, xcb, gx, ga, a2, hs = B["t"]
        T = B["T"]
        bx, by, bgx, bga = banks
        sl = slice(tt * 512, (tt + 1) * 512)
        hr = [self.Hh[kk][tt] for kk in range(DC)]
        self.mm_group(self.bank(bx), [(xt_[:, kk, hl * 128:(hl + 1) * 128], self.hT[:, kk, sl]) for kk in range(DC)], self.PS[bx], [xtk] + hr)
        self.mm_group(self.bank(by), [(yt_[:, kk, hl * 128:(hl + 1) * 128], self.hT[:, kk, sl]) for kk in range(DC)], self.PS[by], [ytk] + hr)
        yield
        if first0:
            k.op("dve", lambda e: e.memset(xpad[:, 0:3], 0.0), writes=[T["xpad"]])
        else:
            k.op("dve", lambda e: e.tensor_copy(xpad[:, 0:3], self.rg_tail[:, i, hd, :]), reads=[self.RGT[i][hd]], writes=[T["xpad"]])
        k.op("act", lambda e: e.activation(xpad[:, 3:515], self.bank(bx), AF.Copy), reads=[self.PS[bx]], writes=[T["xpad"]])
        yield
        k.op("act", lambda e: e.activation(self.rg_tail[:, i, hd, :], xpad[:, 512:515], AF.Copy), reads=[T["xpad"]], writes=[self.RGT[i][hd]])
        cw = lambda j: self.convwT[:, hd, i * 4 + j:i * 4 + j + 1]
        k.op("act", lambda e: e.activation(xc, xpad[:, 3:515], AF.Identity, scale=cw(3), bias=self.convbT[:, hd, i:i + 1]),
             reads=[T["xpad"], C], writes=[T["xc"]])
        yield
        for j in range(3):
            k.op("dve", lambda e: e.scalar_tensor_tensor(xc, xpad[:, j:j + 512], cw(j), xc, ALU.mult, ALU.add),
                 reads=[T["xpad"], T["xc"], C], writes=[T["xc"]])
            if j == 0:
                k.op("act", lambda e: e.activation(ty, self.bank(by), AF.Gelu_apprx_tanh), reads=[self.PS[by]], writes=[T["ty"]])
            yield
        k.op("act", lambda e: e.activation(xcb, xc, AF.Copy), reads=[T["xc"]], writes=[T["xcb"]])
        self.mm_group(self.bank(bga), [(self.gatew[:, i, hd, 128:256], xcb)], self.PS[bga], [T["xcb"], C])
        self.mm_group(self.bank(bgx), [(self.gatew[:, i, hd, 0:128], xcb)], self.PS[bgx], [T["xcb"], C])
        yield
        r = i * 16 + hd * 2
        k.op("act", lambda e: e.activation(ga, self.bank(bga), AF.Sigmoid, bias=self.gbT[:, 0, r + 1:r + 2]), reads=[self.PS[bga], C], writes=[T["ga"]])
        yield
        k.op("act", lambda e: e.activation(a2, ga, AF.Exp, scale=self.cA2[:, hd, i:i + 1]), reads=[T["ga"], C], writes=[T["a2"]])
        yield
        k.op("act", lambda e: e.activation(ga, ga, AF.Exp, scale=self.cA[:, hd, i:i + 1]), reads=[T["ga"], C], writes=[T["ga"]])
        k.op("dve", lambda e: e.tensor_scalar(a2, a2, 1.0, -1.0, ALU.min, ALU.mult), reads=[T["a2"]], writes=[T["a2"]])
        yield
        k.op("act", lambda e: e.activation(gx, self.bank(bgx), AF.Sigmoid, bias=self.gbT[:, 0, r:r + 1]), reads=[self.PS[bgx], C], writes=[T["gx"]])
        yield
        k.op("act", lambda e: e.activation(a2, a2, AF.Sqrt, scale=1.0, bias=1.0), reads=[T["a2"]], writes=[T["a2"]])
        k.op("dve", lambda e: e.tensor_tensor(gx, gx, xc, ALU.mult), reads=[T["gx"], T["xc"]], writes=[T["gx"]])
        yield
        if first0:
            k.op("dve", lambda e: e.memset(a2[:, 0:1], 1.0), reads=[T["a2"]], writes=[T["a2"]])
        k.op("dve", lambda e: e.tensor_tensor(gx, gx, a2, ALU.mult), reads=[T["gx"], T["a2"]], writes=[T["gx"]])
        yield
        if first0:
            k.op("dve", lambda e: e.tensor_tensor_scan(hs, ga, gx, 0.0, ALU.mult, ALU.add), reads=[T["ga"], T["gx"]], writes=[T["hs"]])
        else:
            k.op("dve", lambda e: e.tensor_tensor_scan(hs, ga, gx, self.rg_h[:, i, hd:hd + 1], ALU.mult, ALU.add),
                 reads=[T["ga"], T["gx"], self.RGH[i][hd]], writes=[T["hs"]])
        yield
        k.op("act", lambda e: e.activation(self.rg_h[:, i, hd:hd + 1], hs[:, 511:512], AF.Copy), reads=[T["hs"]], writes=[self.RGH[i][hd]])
        k.op("dve", lambda e: e.tensor_tensor(hy[:, hd, sl], hs, ty, ALU.mult), reads=[T["hs"], T["ty"]], writes=[HY[hd][tt]])
        yield

    def emit_rg(self, l, i, s, first):
        k = self.k
        k.barrier()
        self.emit_norm(l, 1, s)
        ar = self.ar
        ar.reset()
        hy = ar.bf16(DC * TB).rearrange("p (c t) -> p c t", c=DC)
        HY = [[Tk() for _ in range(NT)] for _ in range(DC)]
        BUF = []
        for q in range(2):
            t = [ar.f32(516), ar.f32(512), ar.f32(512), ar.bf16(512), ar.f32(512), ar.f32(512), ar.f32(512), ar.f32(512)]
            BUF.append({"t": t, "T": {n: Tk() for n in ("xpad", "ty", "xc", "xcb", "gx", "ga", "a2", "hs")}})
        winv = self.rg_w_in[i].rearrange("(k p) n -> p k n", p=128)
        for tt in range(NT):
            for hg in range(2):
                yt_, ytk = self.W.get(winv[:, :, hg * 512:(hg + 1) * 512])
                xt_, xtk = self.W.get(winv[:, :, D + hg * 512:D + (hg + 1) * 512])
                for pr in range(2):
                    gens = []
                    for q in range(2):
                        hl = pr * 2 + q
                        hd = hg * 4 + hl
                        gens.append(self._rg_chain(i, hd, hl, tt, first and tt == 0, BUF[q], (q * 4, q * 4 + 1, q * 4 + 2, q * 4 + 3),
                                                   xt_, xtk, yt_, ytk, hy, HY))
                    alive = list(gens)
                    while alive:
                        for g in list(alive):
                            try:
                                next(g)
                            except StopIteration:
                                alive.remove(g)
        wov = self.rg_w_out[i].rearrange("(k p) n -> p k n", p=128)
        self.out_proj(wov, hy, HY, l, s, 1)

    def emit_gl(self, l, kind, s, first):
        k = self.k
        k.barrier()
        self.emit_norm(l, 1, s)
        C = self.CONST
        if kind == "hg":
            H, DVC = 8, 1
            w_in = self.hg_w_in[0]
            qoff, koff, voff, goff = 0, D, 2 * D, 3 * D
            w_out = self.hg_w_out[0]
            groups = [list(range(8))]
            gate_f = AF.Sigmoid
        else:
            H, DVC = 4, 2
            w_in = self.gla_w_in[0]
            qoff, koff, voff, goff, aoff = 0, 512, 1024, 2048, 3072
            w_out = self.gla_w_out[0]
            groups = [[0, 1], [2, 3], [4, 5], [6, 7]]
            gate_f = AF.Silu
        DV = DVC * 128
        HW = H * 128
        winv = w_in.rearrange("(k p) n -> p k n", p=128)
        st = self.st[kind][:].rearrange("p (h v) -> p h v", h=H)
        stb = self.stb[kind][:].rearrange("p (h v) -> p h v", h=H)
        ST, STB = self.ST[kind], self.STB[kind]
        ar = self.ar
        ar.reset()
        oT = ar.bf16(DC * TB).rearrange("p (c t) -> p c t", c=DC)
        OT = [Tk() for _ in range(NT)]
        base_off = ar.off
        qe = ar.bf16(H * 512).rearrange("p (h t) -> p h t", h=H)
        ke = ar.bf16(H * 512).rearrange("p (h t) -> p h t", h=H)
        kd = ar.bf16(H * 512).rearrange("p (h t) -> p h t", h=H)
        QE = [Tk() for _ in range(H)]
        KE = [Tk() for _ in range(H)]
        KD = [Tk() for _ in range(H)]
        t_sig, t_kk, t_b, t_eb, t_enb = [ar.f32(512) for _ in range(5)]
        T = {n: Tk() for n in ("sig", "kk", "b", "eb", "enb", "vtok", "kdtok", "scT", "d", "alow")}
        vtok = [ar.bf16(1024), ar.bf16(1024)]
        kdtok = [ar.bf16(1024), ar.bf16(1024)]
        scT = [ar.bf16(512), ar.bf16(512)]
        VT = [Tk(), Tk()]
        KT = [Tk(), Tk()]
        SC = [Tk(), Tk()]
        dd = ar.f32(H * NCL).rearrange("p (h c) -> p h c", h=H)
        alowT = ar.bf16(512)
        if first:
            k.op("dve", lambda e: e.memset(self.st[kind][:], 0.0), writes=[ST])
            k.op("act", lambda e: e.activation(self.stb[kind][:], self.st[kind][:], AF.Copy), reads=[ST], writes=[STB])
        for tt in range(NT):
            sl = slice(tt * 512, (tt + 1) * 512)
            hr = [self.Hh[kk][tt] for kk in range(DC)]
            if kind == "gla":
                at_, atk = self.W.get(winv[:, :, aoff:aoff + 16])
                self.mm_group(self.bank(6)[0:16, :], [(at_[:, kk, 0:16], self.hT[:, kk, sl]) for kk in range(DC)], self.PS[6], [atk] + hr)
                k.op("act", lambda e: e.activation(alowT[0:16, :], self.bank(6)[0:16, :], AF.Copy), reads=[self.PS[6]], writes=[T["alow"]])
            for hg in range(HW // 512):
                qt_, qtk = self.W.get(winv[:, :, qoff + hg * 512:qoff + (hg + 1) * 512])
                kt_, ktk = self.W.get(winv[:, :, koff + hg * 512:koff + (hg + 1) * 512])
                for hl in range(4):
                    hd = hg * 4 + hl
                    bq, bk, bz = hd % 2, 2 + hd % 2, 4 + hd % 2
                    self.mm_group(self.bank(bq), [(qt_[:, kk, hl * 128:(hl + 1) * 128], self.hT[:, kk, sl]) for kk in range(DC)], self.PS[bq], [qtk] + hr)
                    self.mm_group(self.bank(bk), [(kt_[:, kk, hl * 128:(hl + 1) * 128], self.hT[:, kk, sl]) for kk in range(DC)], self.PS[bk], [ktk] + hr)
                    if kind == "hg":
                        k.op("act", lambda e, bk=bk: e.activation(t_sig, self.bank(bk), AF.Sigmoid), reads=[self.PS[bk]], writes=[T["sig"]])
                        k.op("dve", lambda e, hd=hd: e.tensor_scalar(t_kk, t_sig, self.noml[:, hd, l:l + 1], self.oml[:, hd, l:l + 1], ALU.mult, ALU.add),
                             reads=[T["sig"], C], writes=[T["kk"]])
                        k.op("act", lambda e, hd=hd: e.activation(t_sig, t_sig, AF.Ln, scale=self.oml[:, hd, l:l + 1], bias=self.lb[:, hd, l:l + 1]),
                             reads=[T["sig"], T["kk"], C], writes=[T["sig"]])
                        esc = 1.0
                    else:
                        self.mm_group(self.bank(bz), [(self.gw2[0:16, hd * 128:(hd + 1) * 128], alowT[0:16, :])], self.PS[bz], [T["alow"], C])
                        k.op("act", lambda e, bz=bz, hd=hd: e.activation(t_sig, self.bank(bz), AF.Sigmoid, bias=self.glabT[:, 0, hd:hd + 1]),
                             reads=[self.PS[bz], C], writes=[T["sig"]])
                        k.op("act", lambda e: e.activation(t_sig, t_sig, AF.Ln), reads=[T["sig"]], writes=[T["sig"]])
                        esc = 1.0 / 16.0
                    k.op("dve", lambda e: e.tensor_tensor_scan(t_b, self.cmask[:], t_sig, 0.0, ALU.mult, ALU.add), reads=[T["sig"], C], writes=[T["b"]])
                    k.op("act", lambda e, esc=esc: e.activation(t_eb, t_b, AF.Exp, scale=esc), reads=[T["b"]], writes=[T["eb"]])
                    k.op("act", lambda e, esc=esc: e.activation(t_enb, t_b, AF.Exp, scale=-esc), reads=[T["b"]], writes=[T["enb"]])
                    if kind == "hg":
                        k.op("dve", lambda e, bq=bq, hd=hd: e.tensor_tensor(qe[:, hd, :], self.bank(bq), t_eb, ALU.mult),
                             reads=[self.PS[bq], T["eb"]], writes=[QE[hd]])
                        k.op("dve", lambda e: e.tensor_tensor(t_kk, t_kk, t_enb, ALU.mult), reads=[T["kk"], T["enb"]], writes=[T["kk"]])
                    else:
                        k.op("dve", lambda e, bq=bq, hd=hd: e.scalar_tensor_tensor(qe[:, hd, :], self.bank(bq), 128.0 ** -0.5, t_eb, ALU.mult, ALU.mult),
                             reads=[self.PS[bq], T["eb"]], writes=[QE[hd]])
                        k.op("dve", lambda e, bk=bk: e.tensor_tensor(t_kk, self.bank(bk), t_enb, ALU.mult), reads=[self.PS[bk], T["enb"]], writes=[T["kk"]])
                    k.op("act", lambda e, hd=hd: e.activation(ke[:, hd, :], t_kk, AF.Copy), reads=[T["kk"]], writes=[KE[hd]])
                    k.op("act", lambda e, hd=hd: e.activation(dd[:, hd, :], t_eb.rearrange("p (c t) -> p c t", t=CH)[:, :, CH - 1], AF.Copy),
                         reads=[T["eb"]], writes=[T["d"]])
                    k.op("dve", lambda e, hd=hd: e.tensor_tensor(kd[:, hd, :].rearrange("p (c t) -> p c t", t=CH), t_kk.rearrange("p (c t) -> p c t", t=CH),
                                                                 dd[:, hd, :].unsqueeze(2).to_broadcast([128, NCL, CH]), ALU.mult),
                         reads=[T["kk"], T["d"]], writes=[KD[hd]])
            v0, v0k = self.W.get(winv[:, :, voff:voff + 512])
            v1, v1k = self.W.get(winv[:, :, voff + 512:voff + 1024])
            def pre(cl, q):
                t0 = tt * 512 + cl * CH
                cs = slice(cl * CH, (cl + 1) * CH)
                vp = self.psb[2]
                for half, (vt, vtk) in enumerate(((v0, v0k), (v1, v1k))):
                    self.mm_group(vp[0:CH, half * 512:(half + 1) * 512], [(self.hT[:, kk, t0:t0 + CH], vt[:, kk, :]) for kk in range(DC)],
                                  self.PS[4 + half], [vtk] + hr)
                    yield
                k.op("act", lambda e: e.activation(vtok[q][0:CH, 0:512], vp[0:CH, 0:512], AF.Copy), reads=[self.PS[4]], writes=[VT[q]])
                yield
                k.op("act", lambda e: e.activation(vtok[q][0:CH, 512:1024], vp[0:CH, 512:1024], AF.Copy), reads=[self.PS[5]], writes=[VT[q]])
                kpb = self.bankb(6)
                for hd in range(H):
                    k.op("pe", lambda e: e.transpose(kpb[0:CH, hd * 128:(hd + 1) * 128], kd[:, hd, cs], self.identb[:]),
                         reads=[KD[hd], C], writes=[self.PS[6]], inc=(hd == H - 1))
                yield
                k.op("dve", lambda e: e.tensor_copy(kdtok[q][0:CH, 0:HW], kpb[0:CH, 0:HW]), reads=[self.PS[6]], writes=[KT[q]])
                sp_ = self.bank(7)
                for hd in range(H):
                    k.op("pe", lambda e: e.matmul(sp_[0:CH, hd * CH:(hd + 1) * CH], ke[:, hd, cs], qe[:, hd, cs], start=True, stop=True),
                         reads=[KE[hd], QE[hd]], writes=[self.PS[7]], inc=(hd == H - 1))
                yield
                k.op("dve", lambda e: e.tensor_tensor(scT[q][0:CH, 0:H * CH], sp_[0:CH, 0:H * CH], self.mask8[0:CH, 0:H * CH], ALU.mult),
                     reads=[self.PS[7], C], writes=[SC[q]])
                yield

            def post(cl, q):
                t0 = tt * 512 + cl * CH
                cs = slice(cl * CH, (cl + 1) * CH)
                kvp = self.psb[1]
                for hd in range(H):
                    self.mm_group(kvp[:, hd * DV:(hd + 1) * DV], [(kdtok[q][0:CH, hd * 128:(hd + 1) * 128], vtok[q][0:CH, hd * DV:(hd + 1) * DV])],
                                  self.PS[2 + (hd * DV) // 512], [KT[q], VT[q]])
                yield
                bo = cl % 2
                op_ = self.bank(bo)
                for m in range(8):
                    hd, dvl = m // DVC, m % DVC
                    self.mm_group(op_[:, m * CH:(m + 1) * CH],
                                  [(vtok[q][0:CH, m * 128:(m + 1) * 128], scT[q][0:CH, hd * CH:(hd + 1) * CH]),
                                   (stb[:, hd, dvl * 128:(dvl + 1) * 128], qe[:, hd, cs])],
                                  self.PS[bo], [VT[q], SC[q], STB, QE[hd]])
                    if m == 3:
                        yield
                yield
                k.op("dve", lambda e: e.tensor_tensor(st, st, dd[:, :, cl].unsqueeze(2).to_broadcast([128, H, DV]), ALU.mult),
                     reads=[ST, T["d"]], writes=[ST])
                k.op("act", lambda e: e.activation(oT[:, :, t0:t0 + CH], op_.rearrange("p (m t) -> p m t", m=8), AF.Copy),
                     reads=[self.PS[bo]], writes=[OT[tt]])
                yield
                k.op("dve", lambda e: e.tensor_tensor(self.st[kind][:], self.st[kind][:], kvp[:], ALU.add),
                     reads=[ST, self.PS[2], self.PS[3]], writes=[ST])
                yield
                k.op("act", lambda e: e.activation(self.stb[kind][:], self.st[kind][:], AF.Copy), reads=[ST], writes=[STB])
                yield

            def drive(gens):
                alive = list(gens)
                while alive:
                    for g in list(alive):
                        try:
                            next(g)
                        except StopIteration:
                            alive.remove(g)

            drive([pre(0, 0)])
            for cl in range(NCL):
                gens = [post(cl, cl % 2)]
                if cl + 1 < NCL:
                    gens.append(pre(cl + 1, (cl + 1) % 2))
                drive(gens)
        k.barrier()
        ar.reset(base_off)
        ng = len(groups)
        rs = [[ar.f32(512) for _ in range(NT)] for _ in range(ng)]
        RS = [[Tk() for _ in range(NT)] for _ in range(ng)]
        sgt = [ar.f32(512) for _ in range(2)]
        SGT = [Tk(), Tk()]
        ont = [ar.f32(512) for _ in range(2)]
        ONT = [Tk(), Tk()]
        for tt in range(NT):
            sl = slice(tt * 512, (tt + 1) * 512)
            for m in range(8):
                k.op("act", lambda e, m=m, sl=sl: e.activation(self.sq[:, m, :], oT[:, m, sl], AF.Square), reads=[OT[tt]], writes=[self.SQ[m]])
            for gi, grp in enumerate(groups):
                b = 6 + gi % 2
                self.mm_group(self.bank(b), [(self.onesb[:], self.sq[:, m, :]) for m in grp], self.PS[b], [self.SQ[m] for m in grp] + [C])
                k.op("act", lambda e, b=b, gi=gi, tt=tt, grp=grp: e.activation(rs[gi][tt], self.bank(b), AF.Sqrt, scale=1.0 / (128 * len(grp)), bias=EPS),
                     reads=[self.PS[b]], writes=[RS[gi][tt]])
                k.op("dve", lambda e, gi=gi, tt=tt: e.reciprocal(rs[gi][tt], rs[gi][tt]), reads=[RS[gi][tt]], writes=[RS[gi][tt]])
        OG = [[Tk() for _ in range(NT)] for _ in range(8)]
        rot = 0
        for mg in range(2):
            gt_, gtk = self.W.get(winv[:, :, goff + mg * 512:goff + (mg + 1) * 512])
            for mm in range(4):
                m = mg * 4 + mm
                gi = m // (8 // ng)
                nw = self.hgnwT[:, m, 0:1] if kind == "hg" else self.glanwT[:, 0, (m % 2):(m % 2) + 1]
                for tt in range(NT):
                    sl = slice(tt * 512, (tt + 1) * 512)
                    b = rot % 4
                    r2 = rot % 2
                    rot += 1
                    self.mm_group(self.bank(b), [(gt_[:, kk, mm * 128:(mm + 1) * 128], self.hT[:, kk, sl]) for kk in range(DC)],
                                  self.PS[b], [gtk] + [self.Hh[kk][tt] for kk in range(DC)])
                    k.op("act", lambda e, b=b, r2=r2: e.activation(sgt[r2], self.bank(b), gate_f), reads=[self.PS[b]], writes=[SGT[r2]])
                    k.op("dve", lambda e, m=m, sl=sl, gi=gi, tt=tt, r2=r2: e.tensor_tensor(ont[r2], oT[:, m, sl], rs[gi][tt], ALU.mult),
                         reads=[OT[tt], RS[gi][tt]], writes=[ONT[r2]])
                    k.op("dve", lambda e, m=m, sl=sl, r2=r2, nw=nw: e.scalar_tensor_tensor(oT[:, m, sl], ont[r2], nw, sgt[r2], ALU.mult, ALU.mult),
                         reads=[ONT[r2], SGT[r2], C], writes=[OG[m][tt]])
        wov = w_out.rearrange("(k p) n -> p k n", p=128)
        self.out_proj(wov, oT, OG, l, s, 1)

    def emit_all(self):
        self.out_tks = []
        self.ps_rot = 0
        self.prologue()
        for blk in range(self.nblk):
            s = blk // BLK_PER_SEQ
            first = (blk % BLK_PER_SEQ == 0)
            self.emit_load(blk)
            for l in self.layers:
                if 'ffn0' in self.parts:
                    self.emit_ffn(l, 0, s)
                m = l % 3
                if 'mix' in self.parts:
                    if m == 0:
                        self.emit_rg(l, l // 3, s, first)
                    elif m == 1:
                        self.emit_gl(l, "hg", s, first)
                    else:
                        self.emit_gl(l, "gla", s, first)
                if 'ffn1' in self.parts:
                    self.emit_ffn(l, 1, s)
            self.k.barrier()
            self.emit_store(blk, self.final)
        self.k.wait_all("sp", self.out_tks)

    def alloc(self, name, shape, dtype):
        if name not in self._acache:
            self._acache[name] = self.nc.alloc_sbuf_tensor(name, shape, dtype)
        return self._acache[name]

    def build(self):
        self.k.dry = True
        self.emit_all()
        self.k.dry = False
        self.emit_all()
        assert self.W.pos == len(self.W.plan), (self.W.pos, len(self.W.plan))
        self.k.finish()
        return self.nc


_KEYS = ["ada_w", "ada_b", "norm_w", "final_norm_w", "ffn_w13", "ffn_w2", "rg_w_in", "rg_conv_w", "rg_conv_b", "rg_gate_w",
         "rg_gate_b", "rg_lambda", "rg_w_out", "hg_w_in", "hg_lb_logits", "hg_norm_w", "hg_w_out", "gla_w_in",
         "gla_gate_w2", "gla_gate_b", "gla_norm_w", "gla_w_out"]


def _shared_inputs(inp):
    f = lambda a: np.ascontiguousarray(np.asarray(a, dtype=np.float32))
    sh = {kk: f(inp[kk]) for kk in _KEYS}
    sh["norm_w"] = sh["norm_w"].reshape(DEPTH * 3, D)
    sh["final_norm_w"] = sh["final_norm_w"].reshape(1, D)
    sh["rg_conv_w"] = sh["rg_conv_w"].reshape(8, D)
    sh["rg_gate_b"] = sh["rg_gate_b"].reshape(32, 128)
    sh["gla_gate_w2"] = sh["gla_gate_w2"].reshape(16, 512)
    sh["gla_gate_b"] = sh["gla_gate_b"].reshape(4, 128)
    sh["gla_norm_w"] = sh["gla_norm_w"].reshape(2, 128)
    return sh


LAUNCH_GROUPS = [[0, 1, 2, 3]]
_PROG_CACHE = {}


def _get_prog(layers, first, last):
    key = (tuple(layers), first, last)
    if key not in _PROG_CACHE:
        p = Prog(layers, first, last)
        p.build()
        _PROG_CACHE[key] = p
    return _PROG_CACHE[key]


def kernel(**inp):
    x = np.ascontiguousarray(np.asarray(inp["x"], dtype=np.float32))
    c = np.ascontiguousarray(np.asarray(inp["c"], dtype=np.float32))
    sh = _shared_inputs(inp)
    B = x.shape[0]
    cur = x.reshape(NCORES, SEQ_PER_CORE * SEQ, D)
    cs = c.reshape(NCORES, SEQ_PER_CORE, D)
    for gi, layers in enumerate(LAUNCH_GROUPS):
        p = _get_prog(layers, gi == 0, gi == len(LAUNCH_GROUPS) - 1)
        in_maps = []
        for r in range(NCORES):
            m = dict(sh)
            for kk in ("ada_w", "ffn_w13", "ffn_w2"):
                m[kk] = np.ascontiguousarray(sh[kk][list(layers)])
            m["x"] = np.ascontiguousarray(cur[r])
            m["c"] = np.ascontiguousarray(cs[r])
            in_maps.append(m)
        res = run_bass_kernel_spmd(p.nc, in_maps, core_ids=list(range(NCORES)))
        cur = np.stack([np.asarray(res.results[r]["y"]) for r in range(NCORES)], axis=0)
    return cur.reshape(B, SEQ, D).astype(np.float32)
```

```python
import numpy as np
import concourse.bass as bass
import concourse.mybir as mybir
from concourse.bass_utils import run_bass_kernel_spmd

F32 = mybir.dt.float32
BF16 = mybir.dt.bfloat16
AF = mybir.ActivationFunctionType
ALU = mybir.AluOpType
AX = mybir.AxisListType

D = 1024
DC = 8
DFF = 2816
FC = 22
SEQ = 2048
NCORES = 8
SEQ_PER_CORE = 4
TB = 1024
NT = TB // 512
NBLK = SEQ_PER_CORE * SEQ // TB
BLK_PER_SEQ = SEQ // TB
CH = 64
NCL = 512 // CH
EPS = 1e-6
DEPTH = 4
NS = 4
SLOT = 4096
LIVE = 2
ARENA_WORDS = 16384

SAME_ENGINE_SYNC = True


class SemW:
    def __init__(self, nc, name):
        self.sem = nc.alloc_semaphore(name)
        self.count = 0


class Tk:
    __slots__ = ("writer", "readers")

    def __init__(self):
        self.writer = None
        self.readers = {}


class _Rec:
    def __init__(self):
        self.calls = []

    def __getattr__(self, name):
        def f(*a, **kw):
            self.calls.append((name, a, kw))
            return self
        return f


class K:
    ENGS = ("pe", "act", "dve", "pool", "sp")
    CE = ("pe", "act", "dve", "pool")

    def __init__(self, nc):
        self.nc = nc
        self.dry = False
        self.streams = {e: [] for e in self.ENGS}
        self.esem = {e: SemW(nc, "s_" + e) for e in self.CE}
        self.pending = {e: False for e in self.CE}
        self.waited = {e: {} for e in self.ENGS}
        self.rings = {"sp": [SemW(nc, "dsp%d" % i) for i in range(8)],
                      "pool": [SemW(nc, "dpl%d" % i) for i in range(8)],
                      "act": [SemW(nc, "dac%d" % i) for i in range(4)]}
        self.ridx = {"sp": 0, "pool": 0, "act": 0}
        self.n_instr = {e: 0 for e in self.ENGS}

    def _wait(self, eng, semw, val):
        w = self.waited[eng]
        if w.get(id(semw), 0) >= val:
            return
        w[id(semw)] = val
        sem = semw.sem
        self.streams[eng].append(lambda e, sem=sem, val=val: e.wait_ge(sem, val))

    def _deps(self, eng, reads, writes, is_dma=False):
        for t in reads:
            wr = t.writer
            if wr is not None:
                if wr[2] != eng or is_dma or (SAME_ENGINE_SYNC and eng != "pe"):
                    self._wait(eng, wr[0], wr[1])
        for t in writes:
            wr = t.writer
            if wr is not None and (wr[2] != eng or is_dma):
                self._wait(eng, wr[0], wr[1])
            for en, (s, v) in t.readers.items():
                if en != eng or is_dma:
                    self._wait(eng, s, v)

    def _mark(self, eng, semw, val, reads, writes):
        for t in reads:
            t.readers[eng] = (semw, val)
        for t in writes:
            t.writer = (semw, val, eng)
            t.readers = {}

    def op(self, eng, fn, reads=(), writes=(), inc=True):
        if self.dry:
            return
        self._deps(eng, reads, writes)
        semw = self.esem[eng]
        val = semw.count + 1
        rec = _Rec()
        fn(rec)
        assert len(rec.calls) == 1
        name, a, kw = rec.calls[0]
        if inc:
            semw.count = val
            self.pending[eng] = False
            sem = semw.sem
            self.streams[eng].append(lambda e, name=name, a=a, kw=kw, sem=sem: getattr(e, name)(*a, **kw).then_inc(sem, 1))
        else:
            self.pending[eng] = True
            self.streams[eng].append(lambda e, name=name, a=a, kw=kw: getattr(e, name)(*a, **kw))
        self.n_instr[eng] += 1
        self._mark(eng, semw, val, reads, writes)

    def dma(self, q, out, in_, reads=(), writes=(), **kw):
        if self.dry:
            return
        ring = self.rings[q]
        semw = ring[self.ridx[q]]
        self.ridx[q] = (self.ridx[q] + 1) % len(ring)
        if semw.count > 0:
            self._wait(q, semw, semw.count)
        self._deps(q, reads, writes, is_dma=True)
        semw.count += 16
        val = semw.count
        sem = semw.sem
        self.streams[q].append(
            lambda e, out=out, in_=in_, sem=sem, kw=kw: e.dma_start(out=out, in_=in_, **kw).then_inc(sem, 16))
        self.n_instr[q] += 1
        self._mark("dma%d" % id(semw), semw, val, reads, writes)

    def wait_all(self, eng, toks):
        if self.dry:
            return
        for t in toks:
            if t.writer is not None:
                self._wait(eng, t.writer[0], t.writer[1])

    def barrier(self, engs=("pe", "act", "dve")):
        if self.dry:
            return
        for e in engs:
            assert not self.pending[e]
        for e in engs:
            for o in engs:
                if o != e and self.esem[o].count > 0:
                    self._wait(e, self.esem[o], self.esem[o].count)

    def arena_barrier(self):
        if self.dry:
            return
        assert not self.pending["pe"]
        for e in ("act", "dve"):
            for o in ("pe", "act", "dve"):
                if o != e and self.esem[o].count > 0:
                    self._wait(e, self.esem[o], self.esem[o].count)

    def finish(self):
        nc = self.nc
        for e in self.CE:
            assert not self.pending[e], "engine %s has un-incremented trailing ops" % e
        st = self.streams
        with nc.Block() as block:
            @block.sync
            def _(e):
                for f in st["sp"]:
                    f(e)

            @block.tensor
            def _(e):
                for f in st["pe"]:
                    f(e)

            @block.scalar
            def _(e):
                for f in st["act"]:
                    f(e)

            @block.vector
            def _(e):
                for f in st["dve"]:
                    f(e)

            @block.gpsimd
            def _(e):
                for f in st["pool"]:
                    f(e)


class WStream:
    def __init__(self, k, nc):
        self.k = k
        self.slots = [nc.alloc_sbuf_tensor("wslot%d" % i, [128, SLOT], BF16) for i in range(NS)]
        self.tk = [Tk() for _ in range(NS)]
        self.plan = []
        self.pos = 0
        self.issued = 0

    def _view(self, s, src):
        a, b = src.shape[1], src.shape[2]
        assert a * b <= SLOT
        return self.slots[s][:, 0:a * b].rearrange("p (a b) -> p a b", a=a)

    def get(self, src):
        if self.k.dry:
            self.plan.append(src)
            return self._view(0, src), self.tk[0]
        i = self.pos
        self.pos += 1
        lim = min(len(self.plan), i + NS - LIVE + 1)
        while self.issued < lim:
            j = self.issued
            s = j % NS
            sj = self.plan[j]
            self.k.dma("pool", self._view(s, sj), sj, writes=[self.tk[s]])
            self.issued += 1
        s = i % NS
        return self._view(s, src), self.tk[s]


class Arena:
    def __init__(self, nc):
        self.t = nc.alloc_sbuf_tensor("arena", [128, ARENA_WORDS], F32)
        self.off = 0

    def reset(self, off=0):
        self.off = off

    def f32(self, n):
        a = self.off
        self.off += n
        assert self.off <= ARENA_WORDS, "arena overflow %d" % self.off
        return self.t[:, a:a + n]

    def bf16(self, n):
        w = (n + 1) // 2
        a = self.off
        self.off += w
        assert self.off <= ARENA_WORDS, "arena overflow %d" % self.off
        return self.t[:, a:a + w].bitcast(BF16)[:, 0:n]


class Prog:
    def __init__(self, layers, first_launch, last_launch, nblk=NBLK):
        self.layers = list(layers)
        self.li = {l: i for i, l in enumerate(self.layers)}
        nl = len(self.layers)
        self.parts = ('ffn0', 'mix', 'ffn1')
        self.final = last_launch
        self.nblk = nblk
        nc = bass.Bass("TRN2", target_bir_lowering=False)
        self.nc = nc
        ntok = nblk * TB
        nseq = max(1, ntok // SEQ)
        self.nseq = nseq
        dt = lambda name, shape: nc.dram_tensor(name, shape, F32, kind="ExternalInput").ap()
        self.x = dt("x", [ntok, D])
        self.c = dt("c", [SEQ_PER_CORE, D])
        self.ada_w = dt("ada_w", [nl, D, 9 * D])
        self.ada_b = dt("ada_b", [DEPTH, 9 * D])
        self.norm_w = dt("norm_w", [DEPTH * 3, D])
        self.final_norm_w = dt("final_norm_w", [1, D])
        self.ffn_w13 = dt("ffn_w13", [nl, 2, D, 2 * DFF])
        self.ffn_w2 = dt("ffn_w2", [nl, 2, DFF, D])
        self.rg_w_in = dt("rg_w_in", [2, D, 2 * D])
        self.rg_conv_w = dt("rg_conv_w", [8, D])
        self.rg_conv_b = dt("rg_conv_b", [2, D])
        self.rg_gate_w = dt("rg_gate_w", [2, 8, 128, 256])
        self.rg_gate_b = dt("rg_gate_b", [32, 128])
        self.rg_lambda = dt("rg_lambda", [2, D])
        self.rg_w_out = dt("rg_w_out", [2, D, D])
        self.hg_w_in = dt("hg_w_in", [1, D, 4 * D])
        self.hg_lb_logits = dt("hg_lb_logits", [DEPTH, D])
        self.hg_norm_w = dt("hg_norm_w", [1, D])
        self.hg_w_out = dt("hg_w_out", [1, D, D])
        self.gla_w_in = dt("gla_w_in", [1, D, 3088])
        self.gla_gate_w2 = dt("gla_gate_w2", [16, 512])
        self.gla_gate_b = dt("gla_gate_b", [4, 128])
        self.gla_norm_w = dt("gla_norm_w", [2, 128])
        self.gla_w_out = dt("gla_w_out", [1, D, D])
        self.y = nc.dram_tensor("y", [ntok, D], F32, kind="ExternalOutput").ap()

        self._acache = {}
        self.k = K(nc)
        self.W = WStream(self.k, nc)
        self.ar = Arena(nc)
        sb = nc.alloc_sbuf_tensor
        self.xT = sb("xT", [128, DC, TB], F32)
        self.hT = sb("hT", [128, DC, TB], BF16)
        self.X = [[Tk() for _ in range(NT)] for _ in range(DC)]
        self.Hh = [[Tk() for _ in range(NT)] for _ in range(DC)]
        self.psb = [nc.alloc_psum_tensor("psb%d" % i, [128, 1024], F32) for i in range(4)]
        self.PS = [Tk() for _ in range(8)]
        self.identf = sb("identf", [128, 128], F32)
        self.identb = sb("identb", [128, 128], BF16)
        self.onesb = sb("onesb", [128, 128], BF16)
        self.mask8 = sb("mask8", [CH, 8 * CH], F32)
        self.cmask = sb("cmask", [128, 512], F32)
        self.CONST = Tk()
        self.sq = sb("sq", [128, DC, 512], BF16)
        self.SQ = [Tk() for _ in range(DC)]
        self.ntmp = [sb("ntmp%d" % i, [128, 512], F32) for i in range(2)]
        self.NTMP = [Tk(), Tk()]
        self.rstd = sb("rstd", [128, 512], F32)
        self.RSTD = Tk()
        self.xio = [sb("xio%d" % i, [128, D], F32) for i in range(2)]
        self.XIO = [Tk(), Tk()]
        self.cactT = sb("cactT", [128, DC, SEQ_PER_CORE], BF16)
        self.mods = {l: sb("mods%d" % l, [128, 72, SEQ_PER_CORE], F32) for l in self.layers}
        self.Amod = {l: sb("Amod%d" % l, [128, SEQ_PER_CORE, 3, DC], F32) for l in self.layers}
        self.Gmod = {l: sb("Gmod%d" % l, [128, SEQ_PER_CORE, 3, DC], F32) for l in self.layers}
        self.rg_h = sb("rg_h", [128, 2, DC], F32)
        self.rg_tail = sb("rg_tail", [128, 2, DC, 3], F32)
        self.RGH = [[Tk() for _ in range(DC)] for _ in range(2)]
        self.RGT = [[Tk() for _ in range(DC)] for _ in range(2)]
        self.st = {}
        self.stb = {}
        self.ST = {}
        self.STB = {}
        for kind in ("hg", "gla"):
            self.st[kind] = sb("st_" + kind, [128, 1024], F32)
            self.stb[kind] = sb("stb_" + kind, [128, 1024], BF16)
            self.ST[kind] = Tk()
            self.STB[kind] = Tk()
        self.gatew = sb("gatew", [128, 2, 8, 256], BF16)
        self.gw2 = sb("gw2", [16, 512], BF16)
        self.ps_rot = 0

    def bank(self, i):
        return self.psb[i // 2][:, (i % 2) * 512:(i % 2) * 512 + 512]

    def bankb(self, i):
        return self.psb[i // 2][:, (i % 2) * 512:(i % 2) * 512 + 512].bitcast(BF16)

    def mm_group(self, out, pairs, out_tk, reads, per_reads=None):
        n = len(pairs)
        for i, (lhsT, rhs) in enumerate(pairs):
            rd = list(reads) if i == 0 else []
            if per_reads is not None:
                rd = rd + list(per_reads[i])
            self.k.op("pe", lambda e, lhsT=lhsT, rhs=rhs, i=i: e.matmul(out, lhsT, rhs, start=(i == 0), stop=(i == n - 1)),
                      reads=rd, writes=[out_tk], inc=(i == n - 1))

    def vecT(self, name, src2d, R, Wd, func=None, dtype=F32):
        k = self.k
        nch = Wd // 128
        assert nch * R <= 512
        stage = self.ar.f32(Wd)[0:R, :]
        tk = Tk()
        k.dma("sp", stage, src2d, writes=[tk])
        if func is not None:
            k.op("act", lambda e: e.activation(stage, stage, func), reads=[tk], writes=[tk])
        b = self.ps_rot % 6
        self.ps_rot += 1
        ps = self.bank(b)
        for c in range(nch):
            k.op("pe", lambda e, c=c: e.transpose(ps[:, c * R:(c + 1) * R], stage[:, c * 128:(c + 1) * 128], self.identf[0:R, 0:R]),
                 reads=[tk, self.CONST], writes=[self.PS[b]], inc=(c == nch - 1))
        dst = self.alloc(name, [128, nch, R], dtype)
        k.op("dve", lambda e: e.tensor_copy(dst[:], ps[:, 0:nch * R].rearrange("p (c r) -> p c r", c=nch)),
             reads=[self.PS[b]], writes=[self.CONST])
        return dst

    def prologue(self):
        k, nc = self.k, self.nc
        C = self.CONST
        k.op("pool", lambda e: e.memset(self.identf[:], 0.0), writes=[C])
        k.op("pool", lambda e: e.affine_select(self.identf[:], self.identf[:], pattern=[[-1, 128]], compare_op=ALU.not_equal,
                                               fill=1.0, base=0, channel_multiplier=1), reads=[C], writes=[C])
        k.op("pool", lambda e: e.tensor_copy(self.identb[:], self.identf[:]), reads=[C], writes=[C])
        k.op("pool", lambda e: e.memset(self.onesb[:], 1.0), writes=[C])
        k.op("pool", lambda e: e.memset(self.mask8[:], 1.0), writes=[C])
        for h in range(8):
            k.op("pool", lambda e, h=h: e.affine_select(self.mask8[:, h * CH:(h + 1) * CH], self.mask8[:, h * CH:(h + 1) * CH],
                                                        pattern=[[1, CH]], compare_op=ALU.is_ge, fill=0.0, base=0,
                                                        channel_multiplier=-1), reads=[C], writes=[C])
        k.op("pool", lambda e: e.memset(self.cmask[:], 1.0), writes=[C])
        k.op("pool", lambda e: e.memset(self.cmask[:].rearrange("p (a b) -> p a b", b=CH)[:, :, 0:1], 0.0), reads=[C], writes=[C])
        k.dma("pool", self.gatew[:], self.rg_gate_w.rearrange("i h p j -> p i h j"), writes=[C])
        k.dma("pool", self.gw2[:], self.gla_gate_w2, writes=[C])
        k.barrier(("pe", "act", "dve", "pool"))
        self.ar.reset()
        cT = self.vecT("cTf", self.c, SEQ_PER_CORE, D, func=AF.Silu)
        k.op("dve", lambda e: e.tensor_copy(self.cactT[:], cT[:]), reads=[C], writes=[C])
        self.normwT = self.vecT("normwT", self.norm_w, 12, D)
        self.fnwT = self.vecT("fnwT", self.final_norm_w, 1, D)
        self.convwT = self.vecT("convwT", self.rg_conv_w, 8, D)
        self.convbT = self.vecT("convbT", self.rg_conv_b, 2, D)
        self.lamT = self.vecT("lamT", self.rg_lambda, 2, D)
        self.gbT = self.vecT("gbT", self.rg_gate_b, 32, 128)
        self.lbgT = self.vecT("lbgT", self.hg_lb_logits, DEPTH, D)
        self.hgnwT = self.vecT("hgnwT", self.hg_norm_w, 1, D)
        self.glabT = self.vecT("glabT", self.gla_gate_b, 4, 128)
        self.glanwT = self.vecT("glanwT", self.gla_norm_w, 2, 128)
        self.adabT = {}
        for l in self.layers:
            self.adabT[l] = self.vecT("adabT%d" % l, self.ada_b[l].rearrange("(j p) -> j p", p=128), 72, 128)
        sb = self.alloc
        self.cA = sb("cA", [128, DC, 2], F32)
        t = [sb("pt%d" % i, [128, DC, 4], F32) for i in range(6)]
        v = lambda x: x[:, :, 0:2]
        lam = self.lamT[:]
        k.op("act", lambda e: e.activation(v(t[0]), lam, AF.Abs), reads=[C], writes=[C])
        k.op("act", lambda e: e.activation(v(t[1]), v(t[0]), AF.Exp, scale=-1.0), reads=[C], writes=[C])
        k.op("dve", lambda e: e.tensor_scalar(v(t[2]), v(t[1]), 1.0, None, ALU.add), reads=[C], writes=[C])
        k.op("act", lambda e: e.activation(v(t[3]), v(t[2]), AF.Ln), reads=[C], writes=[C])
        k.op("dve", lambda e: e.tensor_scalar(v(t[2]), v(t[2]), -1.0, 1e-30, ALU.add, ALU.max), reads=[C], writes=[C])
        k.op("dve", lambda e: e.reciprocal(v(t[2]), v(t[2])), reads=[C], writes=[C])
        k.op("dve", lambda e: e.tensor_tensor(v(t[3]), v(t[3]), v(t[1]), ALU.mult), reads=[C], writes=[C])
        k.op("dve", lambda e: e.tensor_tensor(v(t[3]), v(t[3]), v(t[2]), ALU.mult), reads=[C], writes=[C])
        k.op("dve", lambda e: e.tensor_scalar(v(t[4]), lam, -1.0, 0.0, ALU.mult, ALU.max), reads=[C], writes=[C])
        k.op("dve", lambda e: e.tensor_tensor(v(t[4]), v(t[4]), v(t[3]), ALU.add), reads=[C], writes=[C])
        k.op("dve", lambda e: e.tensor_scalar(self.cA[:], v(t[4]), -8.0, None, ALU.mult), reads=[C], writes=[C])
        self.cA2 = sb("cA2", [128, DC, 2], F32)
        k.op("dve", lambda e: e.tensor_scalar(self.cA2[:], v(t[4]), -16.0, None, ALU.mult), reads=[C], writes=[C])
        self.lb = sb("lb", [128, DC, DEPTH], F32)
        self.oml = sb("oml", [128, DC, DEPTH], F32)
        self.noml = sb("noml", [128, DC, DEPTH], F32)
        g = self.lbgT[:]
        mx = sb("lbmx", [128, DC], F32)
        ex = sb("lbex", [128, DC, DEPTH], F32)
        sm = sb("lbsm", [128, DC], F32)
        k.op("dve", lambda e: e.tensor_reduce(mx[:], g, AX.X, ALU.max), reads=[C], writes=[C])
        k.op("dve", lambda e: e.tensor_tensor(ex[:], g, mx[:].unsqueeze(2).to_broadcast([128, DC, DEPTH]), ALU.subtract), reads=[C], writes=[C])
        k.op("act", lambda e: e.activation(ex[:], ex[:], AF.Exp), reads=[C], writes=[C])
        k.op("dve", lambda e: e.tensor_reduce(sm[:], ex[:], AX.X, ALU.add), reads=[C], writes=[C])
        k.op("dve", lambda e: e.reciprocal(sm[:], sm[:]), reads=[C], writes=[C])
        k.op("dve", lambda e: e.memset(self.lb[:], 0.0), reads=[C], writes=[C])
        for l in range(1, DEPTH):
            k.op("dve", lambda e, l=l: e.tensor_tensor(self.lb[:, :, l], self.lb[:, :, l - 1], ex[:, :, l], ALU.add), reads=[C], writes=[C])
        k.op("dve", lambda e: e.tensor_tensor(self.lb[:], self.lb[:], sm[:].unsqueeze(2).to_broadcast([128, DC, DEPTH]), ALU.mult), reads=[C], writes=[C])
        k.op("dve", lambda e: e.tensor_scalar(self.oml[:], self.lb[:], -1.0, 1.0, ALU.mult, ALU.add), reads=[C], writes=[C])
        k.op("dve", lambda e: e.tensor_scalar(self.noml[:], self.oml[:], -1.0, None, ALU.mult), reads=[C], writes=[C])
        for l in self.layers:
            awv = self.ada_w[self.li[l]].rearrange("(k p) n -> p k n", p=128)
            ps = self.bank(7)
            for grp in range(18):
                wt, wtk = self.W.get(awv[:, :, grp * 512:(grp + 1) * 512])
                for jj in range(4):
                    j = grp * 4 + jj
                    self.mm_group(ps[:, j * 4:(j + 1) * 4],
                                  [(wt[:, kk, jj * 128:(jj + 1) * 128], self.cactT[:, kk, :]) for kk in range(DC)],
                                  self.PS[7], [wtk, C])
            md = self.mods[l]
            k.op("dve", lambda e, md=md, l=l: e.tensor_tensor(md[:], ps[:, 0:288].rearrange("p (j s) -> p j s", s=SEQ_PER_CORE),
                                                              self.adabT[l][:, 0, :].unsqueeze(2).to_broadcast([128, 72, SEQ_PER_CORE]), ALU.add),
                 reads=[self.PS[7], C], writes=[C])
            for s in range(SEQ_PER_CORE):
                for sub in range(3):
                    sc = md[:, (3 * sub + 1) * 8:(3 * sub + 2) * 8, s]
                    gt = md[:, (3 * sub + 2) * 8:(3 * sub + 3) * 8, s]
                    k.op("dve", lambda e, sc=sc, l=l, s=s, sub=sub: e.scalar_tensor_tensor(
                        self.Amod[l][:, s, sub, :], sc, 1.0, self.normwT[:, :, l * 3 + sub], ALU.add, ALU.mult), reads=[C], writes=[C])
                    k.op("dve", lambda e, gt=gt, l=l, s=s, sub=sub: e.tensor_scalar(
                        self.Gmod[l][:, s, sub, :], gt, (1.0 if sub == 1 else 0.5), None, ALU.mult), reads=[C], writes=[C])
        k.barrier(("pe", "act", "dve", "pool"))

    def emit_load(self, blk):
        k = self.k
        for t8 in range(TB // 128):
            xi = self.xio[t8 % 2]
            tki = self.XIO[t8 % 2]
            k.dma("sp", xi[:], self.x[blk * TB + t8 * 128: blk * TB + (t8 + 1) * 128, :], writes=[tki])
            tt = t8 // 4
            for half in range(2):
                b = half
                ps = self.bank(b)
                for cc in range(4):
                    c = half * 4 + cc
                    k.op("pe", lambda e, cc=cc, c=c, ps=ps, xi=xi: e.transpose(ps[:, cc * 128:(cc + 1) * 128], xi[:, c * 128:(c + 1) * 128], self.identf[:]),
                         reads=[tki, self.CONST], writes=[self.PS[b]], inc=(cc == 3))
                dst = self.xT[:, half * 4:(half + 1) * 4, t8 * 128:(t8 + 1) * 128]
                eng = "act" if half == 0 else "dve"
                if eng == "act":
                    k.op("act", lambda e, dst=dst, ps=ps: e.activation(dst, ps.rearrange("p (c t) -> p c t", c=4), AF.Copy),
                         reads=[self.PS[b]], writes=[self.X[half * 4 + cc][tt] for cc in range(4)])
                else:
                    k.op("dve", lambda e, dst=dst, ps=ps: e.tensor_copy(dst, ps.rearrange("p (c t) -> p c t", c=4)),
                         reads=[self.PS[b]], writes=[self.X[half * 4 + cc][tt] for cc in range(4)])

    def rstd_tile(self, src_of_c, src_tks, tt, nchunks, bankid, scale, in_psum=False):
        k = self.k
        for c in range(nchunks):
            k.op("act", lambda e, c=c: e.activation(self.sq[:, c, :], src_of_c(c), AF.Square), reads=[src_tks[c]], writes=[self.SQ[c]])
        ps = self.bank(bankid)
        self.mm_group(ps, [(self.onesb[:], self.sq[:, c, :]) for c in range(nchunks)], self.PS[bankid],
                      [self.CONST], per_reads=[[self.SQ[c]] for c in range(nchunks)])
        if in_psum:
            k.op("act", lambda e: e.activation(ps, ps, AF.Sqrt, scale=scale, bias=EPS), reads=[self.PS[bankid]], writes=[self.PS[bankid]])
            k.op("dve", lambda e: e.reciprocal(ps, ps), reads=[self.PS[bankid]], writes=[self.PS[bankid]])
        else:
            k.op("act", lambda e: e.activation(self.rstd[:], ps, AF.Sqrt, scale=scale, bias=EPS), reads=[self.PS[bankid]], writes=[self.RSTD])
            k.op("dve", lambda e: e.reciprocal(self.rstd[:], self.rstd[:]), reads=[self.RSTD], writes=[self.RSTD])

    def emit_store(self, blk, final):
        k = self.k
        for tt in range(NT):
            if final:
                self.rstd_tile(lambda c: self.xT[:, c, tt * 512:(tt + 1) * 512], [self.X[c][tt] for c in range(DC)], tt, DC, 6 + tt, 1.0 / D)
            for t4 in range(4):
                t0 = tt * 512 + t4 * 128
                xo = self.xio[t4 % 2]
                tko = self.XIO[t4 % 2]
                for half in range(2):
                    b = half
                    ps = self.bank(b)
                    for cc in range(4):
                        c = half * 4 + cc
                        if final:
                            nt_ = self.ntmp[cc % 2]
                            ntk = self.NTMP[cc % 2]
                            k.op("dve", lambda e, c=c, nt_=nt_: e.scalar_tensor_tensor(
                                nt_[:, 0:128], self.xT[:, c, t0:t0 + 128], self.fnwT[:, c, 0:1], self.rstd[:, t4 * 128:(t4 + 1) * 128], ALU.mult, ALU.mult),
                                reads=[self.X[c][tt], self.RSTD, self.CONST], writes=[ntk])
                            src = nt_[:, 0:128]
                            rd = [ntk, self.CONST]
                        else:
                            src = self.xT[:, c, t0:t0 + 128]
                            rd = [self.X[c][tt], self.CONST]
                        k.op("pe", lambda e, cc=cc, ps=ps, src=src: e.transpose(ps[:, cc * 128:(cc + 1) * 128], src, self.identf[:]),
                             reads=rd, writes=[self.PS[b]])
                    if half == 0:
                        k.op("act", lambda e, xo=xo, ps=ps: e.activation(xo[:, 0:512], ps, AF.Copy), reads=[self.PS[b]], writes=[tko])
                    else:
                        k.op("dve", lambda e, xo=xo, ps=ps: e.tensor_copy(xo[:, 512:1024], ps), reads=[self.PS[b]], writes=[tko])
                yt = Tk()
                k.dma("sp", self.y[blk * TB + t0: blk * TB + t0 + 128, :], xo[:], reads=[tko], writes=[yt])
                self.out_tks.append(yt)

    def emit_norm_tt(self, l, sub, s, tt):
        k = self.k
        sl = slice(tt * 512, (tt + 1) * 512)
        b = 6 + tt
        self.rstd_tile(lambda c: self.xT[:, c, sl], [self.X[c][tt] for c in range(DC)], tt, DC, b, 1.0 / D, in_psum=True)
        for c in range(DC):
            nt_ = self.ntmp[c % 2]
            ntk = self.NTMP[c % 2]
            k.op("dve", lambda e: e.tensor_tensor(nt_[:], self.xT[:, c, sl], self.bank(b), ALU.mult),
                 reads=[self.X[c][tt], self.PS[b]], writes=[ntk])
            k.op("act", lambda e: e.activation(self.hT[:, c, sl], nt_[:], AF.Identity, scale=self.Amod[l][:, s, sub, c:c + 1],
                                               bias=self.mods[l][:, 3 * sub * 8 + c, s:s + 1]),
                 reads=[ntk, self.CONST], writes=[self.Hh[c][tt]])

    def emit_norm(self, l, sub, s):
        for tt in range(NT):
            self.emit_norm_tt(l, sub, s, tt)

    def x_update(self, ps, psk, l, s, sub, m, tt):
        dst = self.xT[:, m, tt * 512:(tt + 1) * 512]
        self.k.op("dve", lambda e: e.scalar_tensor_tensor(dst, ps, self.Gmod[l][:, s, sub, m:m + 1], dst, ALU.mult, ALU.add),
                  reads=[psk, self.X[m][tt], self.CONST], writes=[self.X[m][tt]])

    def out_proj(self, wov, src, src_tks, l, s, sub, hook=None):
        for tt in range(NT):
            for mg in range(2):
                wt, wtk = self.W.get(wov[:, :, mg * 512:(mg + 1) * 512])
                for mm in range(4):
                    m = mg * 4 + mm
                    b = m % 4
                    ps = self.bank(b)
                    self.mm_group(ps, [(wt[:, kk, mm * 128:(mm + 1) * 128], src[:, kk, tt * 512:(tt + 1) * 512]) for kk in range(DC)],
                                  self.PS[b], [wtk] + [src_tks[kk][tt] for kk in range(DC)])
                    self.x_update(ps, self.PS[b], l, s, sub, m, tt)
            if hook is not None:
                hook(tt)

    def emit_ffn(self, l, f, s, hook=None):
        k = self.k
        sub = 0 if f == 0 else 2
        k.arena_barrier()
        ar = self.ar
        ar.reset()
        aT = ar.bf16(FC * TB).rearrange("p (j t) -> p j t", j=FC)
        AT = [[Tk() for _ in range(NT)] for _ in range(FC)]
        sg = [ar.f32(512) for _ in range(2)]
        SG = [Tk(), Tk()]
        w13v = self.ffn_w13[self.li[l], f].rearrange("(k p) n -> p k n", p=128)
        rot = 0
        for j0 in range(0, FC, 4):
            nj = min(4, FC - j0)
            gt, gtk = self.W.get(w13v[:, :, j0 * 128:(j0 + nj) * 128])
            ut, utk = self.W.get(w13v[:, :, DFF + j0 * 128:DFF + (j0 + nj) * 128])
            for tt in range(NT):
                for jj in range(nj):
                    j = j0 + jj
                    sl = slice(tt * 512, (tt + 1) * 512)
                    bg, bu = (rot % 2) * 2, (rot % 2) * 2 + 1
                    rot += 1
                    hr = [self.Hh[kk][tt] for kk in range(DC)]
                    self.mm_group(self.bank(bg), [(gt[:, kk, jj * 128:(jj + 1) * 128], self.hT[:, kk, sl]) for kk in range(DC)], self.PS[bg], [gtk] + hr)
                    self.mm_group(self.bank(bu), [(ut[:, kk, jj * 128:(jj + 1) * 128], self.hT[:, kk, sl]) for kk in range(DC)], self.PS[bu], [utk] + hr)
                    sgi = rot % 2
                    k.op("act", lambda e: e.activation(sg[sgi], self.bank(bg), AF.Silu), reads=[self.PS[bg]], writes=[SG[sgi]])
                    k.op("dve", lambda e: e.tensor_tensor(aT[:, j, sl], sg[sgi], self.bank(bu), ALU.mult),
                         reads=[SG[sgi], self.PS[bu]], writes=[AT[j][tt]])
        w2v = self.ffn_w2[self.li[l], f].rearrange("(j p) n -> p j n", p=128)
        for tt in range(NT):
            sl = slice(tt * 512, (tt + 1) * 512)
            for mg in range(4):
                wa, wak = self.W.get(w2v[:, 0:11, mg * 256:(mg + 1) * 256])
                wb, wbk = self.W.get(w2v[:, 11:22, mg * 256:(mg + 1) * 256])
                for mm in range(2):
                    m = mg * 2 + mm
                    b = 4 + m % 2
                    pairs = [((wa if j < 11 else wb)[:, j % 11, mm * 128:(mm + 1) * 128], aT[:, j, sl]) for j in range(FC)]
                    self.mm_group(self.bank(b), pairs, self.PS[b], [wak, wbk] + [AT[j][tt] for j in range(FC)])
                    self.x_update(self.bank(b), self.PS[b], l, s, sub, m, tt)
            if hook is not None:
                hook(tt)

    def _rg_chain(self, i, hd, hl, tt, first0, B, banks, xt_, xtk, yt_, ytk, hy, HY):
        k = self.k
        C = self.CONST
        xpad, ty, xc, xcb, gx, ga, a2, hs = B["t"]
        T = B["T"]
        bx, by, bgx, bga = banks
        sl = slice(tt * 512, (tt + 1) * 512)
        hr = [self.Hh[kk][tt] for kk in range(DC)]
        self.mm_group(self.bank(bx), [(xt_[:, kk, hl * 128:(hl + 1) * 128], self.hT[:, kk, sl]) for kk in range(DC)], self.PS[bx], [xtk] + hr)
        self.mm_group(self.bank(by), [(yt_[:, kk, hl * 128:(hl + 1) * 128], self.hT[:, kk, sl]) for kk in range(DC)], self.PS[by], [ytk] + hr)
        yield
        if first0:
            k.op("dve", lambda e: e.memset(xpad[:, 0:3], 0.0), writes=[T["xpad"]])
        else:
            k.op("dve", lambda e: e.tensor_copy(xpad[:, 0:3], self.rg_tail[:, i, hd, :]), reads=[self.RGT[i][hd]], writes=[T["xpad"]])
        k.op("act", lambda e: e.activation(xpad[:, 3:515], self.bank(bx), AF.Copy), reads=[self.PS[bx]], writes=[T["xpad"]])
        yield
        k.op("act", lambda e: e.activation(self.rg_tail[:, i, hd, :], xpad[:, 512:515], AF.Copy), reads=[T["xpad"]], writes=[self.RGT[i][hd]])
        cw = lambda j: self.convwT[:, hd, i * 4 + j:i * 4 + j + 1]
        k.op("act", lambda e: e.activation(xc, xpad[:, 3:515], AF.Identity, scale=cw(3), bias=self.convbT[:, hd, i:i + 1]),
             reads=[T["xpad"], C], writes=[T["xc"]])
        yield
        for j in range(3):
            k.op("dve", lambda e: e.scalar_tensor_tensor(xc, xpad[:, j:j + 512], cw(j), xc, ALU.mult, ALU.add),
                 reads=[T["xpad"], T["xc"], C], writes=[T["xc"]])
            if j == 0:
                k.op("act", lambda e: e.activation(ty, self.bank(by), AF.Gelu_apprx_tanh), reads=[self.PS[by]], writes=[T["ty"]])
            yield
        k.op("act", lambda e: e.activation(xcb, xc, AF.Copy), reads=[T["xc"]], writes=[T["xcb"]])
        self.mm_group(self.bank(bga), [(self.gatew[:, i, hd, 128:256], xcb)], self.PS[bga], [T["xcb"], C])
        self.mm_group(self.bank(bgx), [(self.gatew[:, i, hd, 0:128], xcb)], self.PS[bgx], [T["xcb"], C])
        yield
        r = i * 16 + hd * 2
        k.op("act", lambda e: e.activation(ga, self.bank(bga), AF.Sigmoid, bias=self.gbT[:, 0, r + 1:r + 2]), reads=[self.PS[bga], C], writes=[T["ga"]])
        yield
        k.op("act", lambda e: e.activation(a2, ga, AF.Exp, scale=self.cA2[:, hd, i:i + 1]), reads=[T["ga"], C], writes=[T["a2"]])
        yield
        k.op("act", lambda e: e.activation(ga, ga, AF.Exp, scale=self.cA[:, hd, i:i + 1]), reads=[T["ga"], C], writes=[T["ga"]])
        k.op("dve", lambda e: e.tensor_scalar(a2, a2, 1.0, -1.0, ALU.min, ALU.mult), reads=[T["a2"]], writes=[T["a2"]])
        yield
        k.op("act", lambda e: e.activation(gx, self.bank(bgx), AF.Sigmoid, bias=self.gbT[:, 0, r:r + 1]), reads=[self.PS[bgx], C], writes=[T["gx"]])
        yield
        k.op("act", lambda e: e.activation(a2, a2, AF.Sqrt, scale=1.0, bias=1.0), reads=[T["a2"]], writes=[T["a2"]])
        k.op("dve", lambda e: e.tensor_tensor(gx, gx, xc, ALU.mult), reads=[T["gx"], T["xc"]], writes=[T["gx"]])
        yield
        if first0:
            k.op("dve", lambda e: e.memset(a2[:, 0:1], 1.0), reads=[T["a2"]], writes=[T["a2"]])
        k.op("dve", lambda e: e.tensor_tensor(gx, gx, a2, ALU.mult), reads=[T["gx"], T["a2"]], writes=[T["gx"]])
        yield
        if first0:
            k.op("dve", lambda e: e.tensor_tensor_scan(hs, ga, gx, 0.0, ALU.mult, ALU.add), reads=[T["ga"], T["gx"]], writes=[T["hs"]])
        else:
            k.op("dve", lambda e: e.tensor_tensor_scan(hs, ga, gx, self.rg_h[:, i, hd:hd + 1], ALU.mult, ALU.add),
                 reads=[T["ga"], T["gx"], self.RGH[i][hd]], writes=[T["hs"]])
        yield
        k.op("act", lambda e: e.activation(self.rg_h[:, i, hd:hd + 1], hs[:, 511:512], AF.Copy), reads=[T["hs"]], writes=[self.RGH[i][hd]])
        k.op("dve", lambda e: e.tensor_tensor(hy[:, hd, sl], hs, ty, ALU.mult), reads=[T["hs"], T["ty"]], writes=[HY[hd][tt]])
        yield

    def emit_rg(self, l, i, s, first, hook=None):
        k = self.k
        k.arena_barrier()
        ar = self.ar
        ar.reset()
        hy = ar.bf16(DC * TB).rearrange("p (c t) -> p c t", c=DC)
        HY = [[Tk() for _ in range(NT)] for _ in range(DC)]
        BUF = []
        for q in range(2):
            t = [ar.f32(516), ar.f32(512), ar.f32(512), ar.bf16(512), ar.f32(512), ar.f32(512), ar.f32(512), ar.f32(512)]
            BUF.append({"t": t, "T": {n: Tk() for n in ("xpad", "ty", "xc", "xcb", "gx", "ga", "a2", "hs")}})
        winv = self.rg_w_in[i].rearrange("(k p) n -> p k n", p=128)
        for tt in range(NT):
            for hg in range(2):
                yt_, ytk = self.W.get(winv[:, :, hg * 512:(hg + 1) * 512])
                xt_, xtk = self.W.get(winv[:, :, D + hg * 512:D + (hg + 1) * 512])
                for pr in range(2):
                    gens = []
                    for q in range(2):
                        hl = pr * 2 + q
                        hd = hg * 4 + hl
                        gens.append(self._rg_chain(i, hd, hl, tt, first and tt == 0, BUF[q], (q * 4, q * 4 + 1, q * 4 + 2, q * 4 + 3),
                                                   xt_, xtk, yt_, ytk, hy, HY))
                    alive = list(gens)
                    while alive:
                        for g in list(alive):
                            try:
                                next(g)
                            except StopIteration:
                                alive.remove(g)
        wov = self.rg_w_out[i].rearrange("(k p) n -> p k n", p=128)
        self.out_proj(wov, hy, HY, l, s, 1, hook)

    def emit_gl(self, l, kind, s, first, hook=None):
        k = self.k
        k.arena_barrier()
        C = self.CONST
        if kind == "hg":
            H, DVC = 8, 1
            w_in = self.hg_w_in[0]
            qoff, koff, voff, goff = 0, D, 2 * D, 3 * D
            w_out = self.hg_w_out[0]
            groups = [list(range(8))]
            gate_f = AF.Sigmoid
        else:
            H, DVC = 4, 2
            w_in = self.gla_w_in[0]
            qoff, koff, voff, goff, aoff = 0, 512, 1024, 2048, 3072
            w_out = self.gla_w_out[0]
            groups = [[0, 1], [2, 3], [4, 5], [6, 7]]
            gate_f = AF.Silu
        DV = DVC * 128
        HW = H * 128
        winv = w_in.rearrange("(k p) n -> p k n", p=128)
        st = self.st[kind][:].rearrange("p (h v) -> p h v", h=H)
        stb = self.stb[kind][:].rearrange("p (h v) -> p h v", h=H)
        ST, STB = self.ST[kind], self.STB[kind]
        ar = self.ar
        ar.reset()
        oT = ar.bf16(DC * TB).rearrange("p (c t) -> p c t", c=DC)
        OT = [Tk() for _ in range(NT)]
        base_off = ar.off
        qe = ar.bf16(H * 512).rearrange("p (h t) -> p h t", h=H)
        ke = ar.bf16(H * 512).rearrange("p (h t) -> p h t", h=H)
        kd = ar.bf16(H * 512).rearrange("p (h t) -> p h t", h=H)
        QE = [Tk() for _ in range(H)]
        KE = [Tk() for _ in range(H)]
        KD = [Tk() for _ in range(H)]
        t_sig, t_kk, t_b, t_eb, t_enb = [ar.f32(512) for _ in range(5)]
        T = {n: Tk() for n in ("sig", "kk", "b", "eb", "enb", "vtok", "kdtok", "scT", "d", "alow")}
        vtok = [ar.bf16(1024), ar.bf16(1024)]
        kdtok = [ar.bf16(1024), ar.bf16(1024)]
        scT = [ar.bf16(512), ar.bf16(512)]
        VT = [Tk(), Tk()]
        KT = [Tk(), Tk()]
        SC = [Tk(), Tk()]
        dd = ar.f32(H * NCL).rearrange("p (h c) -> p h c", h=H)
        alowT = ar.bf16(512)
        if first:
            k.op("dve", lambda e: e.memset(self.st[kind][:], 0.0), writes=[ST])
            k.op("act", lambda e: e.activation(self.stb[kind][:], self.st[kind][:], AF.Copy), reads=[ST], writes=[STB])
        for tt in range(NT):
            sl = slice(tt * 512, (tt + 1) * 512)
            hr = [self.Hh[kk][tt] for kk in range(DC)]
            if kind == "gla":
                at_, atk = self.W.get(winv[:, :, aoff:aoff + 16])
                self.mm_group(self.bank(6)[0:16, :], [(at_[:, kk, 0:16], self.hT[:, kk, sl]) for kk in range(DC)], self.PS[6], [atk] + hr)
                k.op("act", lambda e: e.activation(alowT[0:16, :], self.bank(6)[0:16, :], AF.Copy), reads=[self.PS[6]], writes=[T["alow"]])
            for hg in range(HW // 512):
                qt_, qtk = self.W.get(winv[:, :, qoff + hg * 512:qoff + (hg + 1) * 512])
                kt_, ktk = self.W.get(winv[:, :, koff + hg * 512:koff + (hg + 1) * 512])
                for hl in range(4):
                    hd = hg * 4 + hl
                    bq, bk, bz = hd % 2, 2 + hd % 2, 4 + hd % 2
                    self.mm_group(self.bank(bq), [(qt_[:, kk, hl * 128:(hl + 1) * 128], self.hT[:, kk, sl]) for kk in range(DC)], self.PS[bq], [qtk] + hr)
                    self.mm_group(self.bank(bk), [(kt_[:, kk, hl * 128:(hl + 1) * 128], self.hT[:, kk, sl]) for kk in range(DC)], self.PS[bk], [ktk] + hr)
                    if kind == "hg":
                        k.op("act", lambda e, bk=bk: e.activation(t_sig, self.bank(bk), AF.Sigmoid), reads=[self.PS[bk]], writes=[T["sig"]])
                        k.op("dve", lambda e, hd=hd: e.tensor_scalar(t_kk, t_sig, self.noml[:, hd, l:l + 1], self.oml[:, hd, l:l + 1], ALU.mult, ALU.add),
                             reads=[T["sig"], C], writes=[T["kk"]])
                        k.op("act", lambda e, hd=hd: e.activation(t_sig, t_sig, AF.Ln, scale=self.oml[:, hd, l:l + 1], bias=self.lb[:, hd, l:l + 1]),
                             reads=[T["sig"], T["kk"], C], writes=[T["sig"]])
                        esc = 1.0
                    else:
                        self.mm_group(self.bank(bz), [(self.gw2[0:16, hd * 128:(hd + 1) * 128], alowT[0:16, :])], self.PS[bz], [T["alow"], C])
                        k.op("act", lambda e, bz=bz, hd=hd: e.activation(t_sig, self.bank(bz), AF.Sigmoid, bias=self.glabT[:, 0, hd:hd + 1]),
                             reads=[self.PS[bz], C], writes=[T["sig"]])
                        k.op("act", lambda e: e.activation(t_sig, t_sig, AF.Ln), reads=[T["sig"]], writes=[T["sig"]])
                        esc = 1.0 / 16.0
                    k.op("dve", lambda e: e.tensor_tensor_scan(t_b, self.cmask[:], t_sig, 0.0, ALU.mult, ALU.add), reads=[T["sig"], C], writes=[T["b"]])
                    k.op("act", lambda e, esc=esc: e.activation(t_eb, t_b, AF.Exp, scale=esc), reads=[T["b"]], writes=[T["eb"]])
                    k.op("act", lambda e, esc=esc: e.activation(t_enb, t_b, AF.Exp, scale=-esc), reads=[T["b"]], writes=[T["enb"]])
                    if kind == "hg":
                        k.op("dve", lambda e, bq=bq, hd=hd: e.tensor_tensor(qe[:, hd, :], self.bank(bq), t_eb, ALU.mult),
                             reads=[self.PS[bq], T["eb"]], writes=[QE[hd]])
                        k.op("dve", lambda e: e.tensor_tensor(t_kk, t_kk, t_enb, ALU.mult), reads=[T["kk"], T["enb"]], writes=[T["kk"]])
                    else:
                        k.op("dve", lambda e, bq=bq, hd=hd: e.scalar_tensor_tensor(qe[:, hd, :], self.bank(bq), 128.0 ** -0.5, t_eb, ALU.mult, ALU.mult),
                             reads=[self.PS[bq], T["eb"]], writes=[QE[hd]])
                        k.op("dve", lambda e, bk=bk: e.tensor_tensor(t_kk, self.bank(bk), t_enb, ALU.mult), reads=[self.PS[bk], T["enb"]], writes=[T["kk"]])
                    k.op("act", lambda e, hd=hd: e.activation(ke[:, hd, :], t_kk, AF.Copy), reads=[T["kk"]], writes=[KE[hd]])
                    k.op("act", lambda e, hd=hd: e.activation(dd[:, hd, :], t_eb.rearrange("p (c t) -> p c t", t=CH)[:, :, CH - 1], AF.Copy),
                         reads=[T["eb"]], writes=[T["d"]])
                    k.op("dve", lambda e, hd=hd: e.tensor_tensor(kd[:, hd, :].rearrange("p (c t) -> p c t", t=CH), t_kk.rearrange("p (c t) -> p c t", t=CH),
                                                                 dd[:, hd, :].unsqueeze(2).to_broadcast([128, NCL, CH]), ALU.mult),
                         reads=[T["kk"], T["d"]], writes=[KD[hd]])
            v0, v0k = self.W.get(winv[:, :, voff:voff + 512])
            v1, v1k = self.W.get(winv[:, :, voff + 512:voff + 1024])
            def pre(cl, q):
                t0 = tt * 512 + cl * CH
                cs = slice(cl * CH, (cl + 1) * CH)
                vp = self.psb[2]
                for half, (vt, vtk) in enumerate(((v0, v0k), (v1, v1k))):
                    self.mm_group(vp[0:CH, half * 512:(half + 1) * 512], [(self.hT[:, kk, t0:t0 + CH], vt[:, kk, :]) for kk in range(DC)],
                                  self.PS[4 + half], [vtk] + hr)
                    yield
                k.op("act", lambda e: e.activation(vtok[q][0:CH, 0:512], vp[0:CH, 0:512], AF.Copy), reads=[self.PS[4]], writes=[VT[q]])
                yield
                k.op("act", lambda e: e.activation(vtok[q][0:CH, 512:1024], vp[0:CH, 512:1024], AF.Copy), reads=[self.PS[5]], writes=[VT[q]])
                kpb = self.bankb(6)
                for hd in range(H):
                    k.op("pe", lambda e: e.transpose(kpb[0:CH, hd * 128:(hd + 1) * 128], kd[:, hd, cs], self.identb[:]),
                         reads=[KD[hd], C], writes=[self.PS[6]], inc=(hd == H - 1))
                yield
                k.op("dve", lambda e: e.tensor_copy(kdtok[q][0:CH, 0:HW], kpb[0:CH, 0:HW]), reads=[self.PS[6]], writes=[KT[q]])
                sp_ = self.bank(7)
                for hd in range(H):
                    k.op("pe", lambda e: e.matmul(sp_[0:CH, hd * CH:(hd + 1) * CH], ke[:, hd, cs], qe[:, hd, cs], start=True, stop=True),
                         reads=[KE[hd], QE[hd]], writes=[self.PS[7]], inc=(hd == H - 1))
                yield
                k.op("dve", lambda e: e.tensor_tensor(scT[q][0:CH, 0:H * CH], sp_[0:CH, 0:H * CH], self.mask8[0:CH, 0:H * CH], ALU.mult),
                     reads=[self.PS[7], C], writes=[SC[q]])
                yield

            def post(cl, q):
                t0 = tt * 512 + cl * CH
                cs = slice(cl * CH, (cl + 1) * CH)
                kvp = self.psb[1]
                for hd in range(H):
                    self.mm_group(kvp[:, hd * DV:(hd + 1) * DV], [(kdtok[q][0:CH, hd * 128:(hd + 1) * 128], vtok[q][0:CH, hd * DV:(hd + 1) * DV])],
                                  self.PS[2 + (hd * DV) // 512], [KT[q], VT[q]])
                yield
                bo = cl % 2
                op_ = self.bank(bo)
                for m in range(8):
                    hd, dvl = m // DVC, m % DVC
                    self.mm_group(op_[:, m * CH:(m + 1) * CH],
                                  [(vtok[q][0:CH, m * 128:(m + 1) * 128], scT[q][0:CH, hd * CH:(hd + 1) * CH]),
                                   (stb[:, hd, dvl * 128:(dvl + 1) * 128], qe[:, hd, cs])],
                                  self.PS[bo], [VT[q], SC[q], STB, QE[hd]])
                    if m == 3:
                        yield
                yield
                k.op("dve", lambda e: e.tensor_tensor(st, st, dd[:, :, cl].unsqueeze(2).to_broadcast([128, H, DV]), ALU.mult),
                     reads=[ST, T["d"]], writes=[ST])
                k.op("act", lambda e: e.activation(oT[:, :, t0:t0 + CH], op_.rearrange("p (m t) -> p m t", m=8), AF.Copy),
                     reads=[self.PS[bo]], writes=[OT[tt]])
                yield
                k.op("dve", lambda e: e.tensor_tensor(self.st[kind][:], self.st[kind][:], kvp[:], ALU.add),
                     reads=[ST, self.PS[2], self.PS[3]], writes=[ST])
                yield
                k.op("act", lambda e: e.activation(self.stb[kind][:], self.st[kind][:], AF.Copy), reads=[ST], writes=[STB])
                yield

            def drive(gens):
                alive = list(gens)
                while alive:
                    for g in list(alive):
                        try:
                            next(g)
                        except StopIteration:
                            alive.remove(g)

            drive([pre(0, 0)])
            for cl in range(NCL):
                gens = [post(cl, cl % 2)]
                if cl + 1 < NCL:
                    gens.append(pre(cl + 1, (cl + 1) % 2))
                drive(gens)
        k.barrier()
        ar.reset(base_off)
        ng = len(groups)
        rs = [[ar.f32(512) for _ in range(NT)] for _ in range(ng)]
        RS = [[Tk() for _ in range(NT)] for _ in range(ng)]
        sgt = [ar.f32(512) for _ in range(2)]
        SGT = [Tk(), Tk()]
        ont = [ar.f32(512) for _ in range(2)]
        ONT = [Tk(), Tk()]
        for tt in range(NT):
            sl = slice(tt * 512, (tt + 1) * 512)
            for m in range(8):
                k.op("act", lambda e, m=m, sl=sl: e.activation(self.sq[:, m, :], oT[:, m, sl], AF.Square), reads=[OT[tt]], writes=[self.SQ[m]])
            for gi, grp in enumerate(groups):
                b = 6 + gi % 2
                self.mm_group(self.bank(b), [(self.onesb[:], self.sq[:, m, :]) for m in grp], self.PS[b], [self.SQ[m] for m in grp] + [C])
                k.op("act", lambda e, b=b, gi=gi, tt=tt, grp=grp: e.activation(rs[gi][tt], self.bank(b), AF.Sqrt, scale=1.0 / (128 * len(grp)), bias=EPS),
                     reads=[self.PS[b]], writes=[RS[gi][tt]])
                k.op("dve", lambda e, gi=gi, tt=tt: e.reciprocal(rs[gi][tt], rs[gi][tt]), reads=[RS[gi][tt]], writes=[RS[gi][tt]])
        OG = [[Tk() for _ in range(NT)] for _ in range(8)]
        rot = 0
        for mg in range(2):
            gt_, gtk = self.W.get(winv[:, :, goff + mg * 512:goff + (mg + 1) * 512])
            for mm in range(4):
                m = mg * 4 + mm
                gi = m // (8 // ng)
                nw = self.hgnwT[:, m, 0:1] if kind == "hg" else self.glanwT[:, 0, (m % 2):(m % 2) + 1]
                for tt in range(NT):
                    sl = slice(tt * 512, (tt + 1) * 512)
                    b = rot % 4
                    r2 = rot % 2
                    rot += 1
                    self.mm_group(self.bank(b), [(gt_[:, kk, mm * 128:(mm + 1) * 128], self.hT[:, kk, sl]) for kk in range(DC)],
                                  self.PS[b], [gtk] + [self.Hh[kk][tt] for kk in range(DC)])
                    k.op("act", lambda e, b=b, r2=r2: e.activation(sgt[r2], self.bank(b), gate_f), reads=[self.PS[b]], writes=[SGT[r2]])
                    k.op("dve", lambda e, m=m, sl=sl, gi=gi, tt=tt, r2=r2: e.tensor_tensor(ont[r2], oT[:, m, sl], rs[gi][tt], ALU.mult),
                         reads=[OT[tt], RS[gi][tt]], writes=[ONT[r2]])
                    k.op("dve", lambda e, m=m, sl=sl, r2=r2, nw=nw: e.scalar_tensor_tensor(oT[:, m, sl], ont[r2], nw, sgt[r2], ALU.mult, ALU.mult),
                         reads=[ONT[r2], SGT[r2], C], writes=[OG[m][tt]])
        wov = w_out.rearrange("(k p) n -> p k n", p=128)
        self.out_proj(wov, oT, OG, l, s, 1, hook)

    def emit_all(self):
        self.out_tks = []
        self.ps_rot = 0
        self.prologue()
        for blk in range(self.nblk):
            s = blk // BLK_PER_SEQ
            first = (blk % BLK_PER_SEQ == 0)
            self.emit_load(blk)
            subs = []
            for l in self.layers:
                if 'ffn0' in self.parts:
                    subs.append(("ffn", l, 0))
                if 'mix' in self.parts:
                    subs.append(("mix", l, 1))
                if 'ffn1' in self.parts:
                    subs.append(("ffn", l, 2))
            if subs:
                self.emit_norm(subs[0][1], subs[0][2], s)
            for si, (kind_, l, sub) in enumerate(subs):
                if si + 1 < len(subs):
                    nl_, nsub = subs[si + 1][1], subs[si + 1][2]
                    hook = (lambda tt, nl_=nl_, nsub=nsub: self.emit_norm_tt(nl_, nsub, s, tt))
                else:
                    hook = None
                if kind_ == "ffn":
                    self.emit_ffn(l, 0 if sub == 0 else 1, s, hook)
                else:
                    m = l % 3
                    if m == 0:
                        self.emit_rg(l, l // 3, s, first, hook)
                    elif m == 1:
                        self.emit_gl(l, "hg", s, first, hook)
                    else:
                        self.emit_gl(l, "gla", s, first, hook)
            self.k.barrier()
            self.emit_store(blk, self.final)
        self.k.wait_all("sp", self.out_tks)

    def alloc(self, name, shape, dtype):
        if name not in self._acache:
            self._acache[name] = self.nc.alloc_sbuf_tensor(name, shape, dtype)
        return self._acache[name]

    def build(self):
        self.k.dry = True
        self.emit_all()
        self.k.dry = False
        self.emit_all()
        assert self.W.pos == len(self.W.plan), (self.W.pos, len(self.W.plan))
        self.k.finish()
        return self.nc


_KEYS = ["ada_w", "ada_b", "norm_w", "final_norm_w", "ffn_w13", "ffn_w2", "rg_w_in", "rg_conv_w", "rg_conv_b", "rg_gate_w",
         "rg_gate_b", "rg_lambda", "rg_w_out", "hg_w_in", "hg_lb_logits", "hg_norm_w", "hg_w_out", "gla_w_in",
         "gla_gate_w2", "gla_gate_b", "gla_norm_w", "gla_w_out"]


def _shared_inputs(inp):
    f = lambda a: np.ascontiguousarray(np.asarray(a, dtype=np.float32))
    sh = {kk: f(inp[kk]) for kk in _KEYS}
    sh["norm_w"] = sh["norm_w"].reshape(DEPTH * 3, D)
    sh["final_norm_w"] = sh["final_norm_w"].reshape(1, D)
    sh["rg_conv_w"] = sh["rg_conv_w"].reshape(8, D)
    sh["rg_gate_b"] = sh["rg_gate_b"].reshape(32, 128)
    sh["gla_gate_w2"] = sh["gla_gate_w2"].reshape(16, 512)
    sh["gla_gate_b"] = sh["gla_gate_b"].reshape(4, 128)
    sh["gla_norm_w"] = sh["gla_norm_w"].reshape(2, 128)
    return sh


LAUNCH_GROUPS = [[0, 1, 2, 3]]
_PROG_CACHE = {}


def _get_prog(layers, first, last):
    key = (tuple(layers), first, last)
    if key not in _PROG_CACHE:
        p = Prog(layers, first, last)
        p.build()
        _PROG_CACHE[key] = p
    return _PROG_CACHE[key]


def kernel(**inp):
    x = np.ascontiguousarray(np.asarray(inp["x"], dtype=np.float32))
    c = np.ascontiguousarray(np.asarray(inp["c"], dtype=np.float32))
    sh = _shared_inputs(inp)
    B = x.shape[0]
    cur = x.reshape(NCORES, SEQ_PER_CORE * SEQ, D)
    cs = c.reshape(NCORES, SEQ_PER_CORE, D)
    for gi, layers in enumerate(LAUNCH_GROUPS):
        p = _get_prog(layers, gi == 0, gi == len(LAUNCH_GROUPS) - 1)
        in_maps = []
        for r in range(NCORES):
            m = dict(sh)
            for kk in ("ada_w", "ffn_w13", "ffn_w2"):
                m[kk] = np.ascontiguousarray(sh[kk][list(layers)])
            m["x"] = np.ascontiguousarray(cur[r])
            m["c"] = np.ascontiguousarray(cs[r])
            in_maps.append(m)
        res = run_bass_kernel_spmd(p.nc, in_maps, core_ids=list(range(NCORES)))
        cur = np.stack([np.asarray(res.results[r]["y"]) for r in range(NCORES)], axis=0)
    return cur.reshape(B, SEQ, D).astype(np.float32)
```
